# Optimizing a Trainium2 kernel written in Bass

```python
import math
import jax, jax.numpy as jnp
from jax import lax
import numpy as np

D_MODEL = 2048
BATCH = 4
SEQ = 2048
DEPTH = 1

HEAD_DIM = 128
ROPE_DIM = HEAD_DIM // 4
ROPE_THETA = 500000.0
N_TOTAL_HEADS = D_MODEL // HEAD_DIM
N_NSA_HEADS = N_TOTAL_HEADS // 2
N_NSA_KV = 2
NSA_GROUP = N_NSA_HEADS // N_NSA_KV
CMP_LEN = 32
CMP_STRIDE = 16
CMP_HIDDEN = 256
SLC_LEN = 64
SLC_TOP = 16
WINDOW = 512
N_DIFF_HEADS = N_TOTAL_HEADS // 4
D_FF = 5632
CONV_W = 3
PLE_DIM = 256
Q_BLOCK = 128
SLC_Q_BLOCK = 64
EPS = 1e-6

NSA_WIDTH = N_NSA_HEADS * HEAD_DIM
NSA_KV_WIDTH = N_NSA_KV * HEAD_DIM
DIFF_QK_WIDTH = N_DIFF_HEADS * 2 * HEAD_DIM
DIFF_V_WIDTH = N_DIFF_HEADS * 2 * HEAD_DIM
MIX_WIDTH = NSA_WIDTH + DIFF_V_WIDTH
IN_SPLITS = (NSA_WIDTH,) + (NSA_KV_WIDTH,) * 6 + (3 * N_NSA_HEADS, DIFF_QK_WIDTH, DIFF_QK_WIDTH, DIFF_V_WIDTH)
IN_WIDTH = sum(IN_SPLITS)

kernel_name = 'hybrid_nsa_diffattn_convffn_ple'


def rmsnorm(x, g):
    xf = x.astype(jnp.float32)
    y = xf * lax.rsqrt(jnp.mean(xf * xf, axis=-1, keepdims=True) + EPS)
    return (y * g.astype(jnp.float32)).astype(x.dtype)


def rope_tables(seq):
    inv = 1.0 / (ROPE_THETA ** (jnp.arange(0, ROPE_DIM, 2, dtype=jnp.float32) / ROPE_DIM))
    ang = jnp.arange(seq, dtype=jnp.float32)[:, None] * inv[None, :]
    return jnp.cos(ang), jnp.sin(ang)


def partial_rope(x, cos, sin):
    c = cos.astype(x.dtype)
    s = sin.astype(x.dtype)
    half = ROPE_DIM // 2
    x1, x2, xp = x[..., :half], x[..., half:ROPE_DIM], x[..., ROPE_DIM:]
    return jnp.concatenate([x1 * c - x2 * s, x2 * c + x1 * s, xp], axis=-1)


def masked_softmax(s, mask):
    s = jnp.where(mask, s.astype(jnp.float32), -1e30)
    p = jax.nn.softmax(s, axis=-1)
    return jnp.where(mask, p, 0.0)


def compress_blocks(blocks, pos, w1, w2):
    b = (blocks + pos.astype(blocks.dtype)).reshape(*blocks.shape[:-2], CMP_LEN * HEAD_DIM)
    return jax.nn.gelu(b @ w1) @ w2


def nsa_attention(q, kc, vc, ks, vs, kw, vw, gates, cmp_k_pos, cmp_k_w1, cmp_k_w2,
                  cmp_v_pos, cmp_v_w1, cmp_v_w2, cos, sin):
    B, S = q.shape[0], q.shape[1]
    scale = HEAD_DIM ** -0.5
    q = partial_rope(q.reshape(B, S, N_NSA_KV, NSA_GROUP, HEAD_DIM).transpose(0, 2, 3, 1, 4), cos, sin)

    def kv_layout(t):
        return t.reshape(B, S, N_NSA_KV, HEAD_DIM).transpose(0, 2, 1, 3)

    kc, ks, kw = (partial_rope(kv_layout(t), cos, sin) for t in (kc, ks, kw))
    vc, vs, vw = (kv_layout(t) for t in (vc, vs, vw))
    t_pos = jnp.arange(S, dtype=jnp.int32)

    n_cmp = (S - CMP_LEN) // CMP_STRIDE + 1
    cmp_starts = np.arange(n_cmp, dtype=np.int32) * CMP_STRIDE
    blk_idx = cmp_starts[:, None] + np.arange(CMP_LEN, dtype=np.int32)[None, :]
    k_cmp = compress_blocks(kc[:, :, blk_idx], cmp_k_pos, cmp_k_w1, cmp_k_w2)
    v_cmp = compress_blocks(vc[:, :, blk_idx], cmp_v_pos, cmp_v_w1, cmp_v_w2)
    cmp_mask = jnp.asarray(cmp_starts + CMP_LEN - 1)[None, :] <= t_pos[:, None]
    s_cmp = jnp.einsum('bhgqd,bhcd->bhgqc', q, k_cmp) * scale
    p_cmp = masked_softmax(s_cmp, cmp_mask)
    o_cmp = jnp.einsum('bhgqc,bhcd->bhgqd', p_cmp.astype(v_cmp.dtype), v_cmp)

    n_sel = S // SLC_LEN
    n_top = min(SLC_TOP, n_sel)
    sel_starts = np.arange(n_sel, dtype=np.int32) * SLC_LEN
    overlap = np.clip(np.minimum(cmp_starts[:, None] + CMP_LEN, sel_starts[None, :] + SLC_LEN)
                      - np.maximum(cmp_starts[:, None], sel_starts[None, :]), 0, None).astype(np.float32) / CMP_LEN
    p_slc = jnp.einsum('bhgqc,cn->bhqn', p_cmp, jnp.asarray(overlap))
    cur = t_pos // SLC_LEN
    j = jnp.arange(n_sel, dtype=jnp.int32)
    forced = (j[None, :] == 0) | (j[None, :] == cur[:, None]) | (j[None, :] == cur[:, None] - 1)
    future = j[None, :] > cur[:, None]
    sel_score = jnp.where(forced, 1e4, jnp.where(future, -1e4, p_slc))
    _, sel_idx = lax.top_k(sel_score, n_top)

    ks_blk = ks.reshape(B, N_NSA_KV, n_sel, SLC_LEN, HEAD_DIM)
    vs_blk = vs.reshape(B, N_NSA_KV, n_sel, SLC_LEN, HEAD_DIM)
    nqs = S // SLC_Q_BLOCK
    q_sb = q.reshape(B, N_NSA_KV, NSA_GROUP, nqs, SLC_Q_BLOCK, HEAD_DIM).transpose(3, 0, 1, 2, 4, 5)
    idx_sb = sel_idx.reshape(B, N_NSA_KV, nqs, SLC_Q_BLOCK, n_top).transpose(2, 0, 1, 3, 4)
    slc_starts = jnp.arange(nqs, dtype=jnp.int32) * SLC_Q_BLOCK
    bi = jnp.arange(B)[:, None, None, None]
    hi = jnp.arange(N_NSA_KV)[None, :, None, None]
    n_keys = n_top * SLC_LEN

    def slc_block(args):
        qb, ib, start = args
        kg = ks_blk[bi, hi, ib].reshape(B, N_NSA_KV, SLC_Q_BLOCK, n_keys, HEAD_DIM)
        vg = vs_blk[bi, hi, ib].reshape(B, N_NSA_KV, SLC_Q_BLOCK, n_keys, HEAD_DIM)
        s = jnp.einsum('bhgqd,bhqkd->bhgqk', qb, kg) * scale
        kpos = (ib[..., None] * SLC_LEN + jnp.arange(SLC_LEN, dtype=jnp.int32)).reshape(B, N_NSA_KV, SLC_Q_BLOCK, n_keys)
        qpos = start + jnp.arange(SLC_Q_BLOCK, dtype=jnp.int32)
        mask = (kpos <= qpos[None, None, :, None])[:, :, None]
        p = masked_softmax(s, mask)
        return jnp.einsum('bhgqk,bhqkd->bhgqd', p.astype(vg.dtype), vg)

    o_slc = lax.map(slc_block, (q_sb, idx_sb, slc_starts))
    o_slc = o_slc.transpose(1, 2, 3, 0, 4, 5).reshape(B, N_NSA_KV, NSA_GROUP, S, HEAD_DIM)

    span = WINDOW + Q_BLOCK
    kw_pad = jnp.pad(kw, ((0, 0), (0, 0), (WINDOW, 0), (0, 0)))
    vw_pad = jnp.pad(vw, ((0, 0), (0, 0), (WINDOW, 0), (0, 0)))
    nqw = S // Q_BLOCK
    q_wb = q.reshape(B, N_NSA_KV, NSA_GROUP, nqw, Q_BLOCK, HEAD_DIM).transpose(3, 0, 1, 2, 4, 5)
    win_starts = jnp.arange(nqw, dtype=jnp.int32) * Q_BLOCK

    def win_block(args):
        qb, start = args
        kb = lax.dynamic_slice_in_dim(kw_pad, start, span, axis=2)
        vb = lax.dynamic_slice_in_dim(vw_pad, start, span, axis=2)
        s = jnp.einsum('bhgqd,bhkd->bhgqk', qb, kb) * scale
        qpos = start + jnp.arange(Q_BLOCK, dtype=jnp.int32)
        kpos = start - WINDOW + jnp.arange(span, dtype=jnp.int32)
        dist = qpos[:, None] - kpos[None, :]
        mask = (dist >= 0) & (dist < WINDOW) & (kpos[None, :] >= 0)
        p = masked_softmax(s, mask)
        return jnp.einsum('bhgqk,bhkd->bhgqd', p.astype(vb.dtype), vb)

    o_win = lax.map(win_block, (q_wb, win_starts))
    o_win = o_win.transpose(1, 2, 3, 0, 4, 5).reshape(B, N_NSA_KV, NSA_GROUP, S, HEAD_DIM)

    g = jax.nn.sigmoid(gates.astype(jnp.float32)).reshape(B, S, 3, N_NSA_KV, NSA_GROUP).transpose(2, 0, 3, 4, 1)[..., None]
    o = g[0] * o_cmp + g[1] * o_slc + g[2] * o_win
    return o.transpose(0, 3, 1, 2, 4).reshape(B, S, NSA_WIDTH).astype(q.dtype)


def diff_attention(q, k, v, lq1, lk1, lq2, lk2, subln, lambda_init, cos, sin):
    B, S = q.shape[0], q.shape[1]
    scale = HEAD_DIM ** -0.5
    q = partial_rope(q.reshape(B, S, N_DIFF_HEADS, 2, HEAD_DIM).transpose(0, 2, 3, 1, 4), cos, sin)
    k = partial_rope(k.reshape(B, S, N_DIFF_HEADS, 2, HEAD_DIM).transpose(0, 2, 3, 1, 4), cos, sin)
    v = v.reshape(B, S, N_DIFF_HEADS, 2 * HEAD_DIM).transpose(0, 2, 1, 3)
    f32 = jnp.float32
    lam = (jnp.exp(jnp.sum(lq1.astype(f32) * lk1.astype(f32)))
           - jnp.exp(jnp.sum(lq2.astype(f32) * lk2.astype(f32))) + lambda_init)
    nqb = S // Q_BLOCK
    q_b = q.reshape(B, N_DIFF_HEADS, 2, nqb, Q_BLOCK, HEAD_DIM).transpose(3, 0, 1, 2, 4, 5)
    starts = jnp.arange(nqb, dtype=jnp.int32) * Q_BLOCK
    kpos = jnp.arange(S, dtype=jnp.int32)

    def blk(args):
        qb, start = args
        s = jnp.einsum('bhmqd,bhmkd->bhmqk', qb, k) * scale
        mask = kpos[None, :] <= (start + jnp.arange(Q_BLOCK, dtype=jnp.int32))[:, None]
        p = masked_softmax(s, mask)
        a = p[:, :, 0] - lam * p[:, :, 1]
        return jnp.einsum('bhqk,bhkd->bhqd', a.astype(v.dtype), v)

    o = lax.map(blk, (q_b, starts))
    o = o.transpose(1, 0, 3, 2, 4).reshape(B, S, N_DIFF_HEADS, 2 * HEAD_DIM)
    o = rmsnorm(o, subln) * (1.0 - lambda_init)
    return o.reshape(B, S, DIFF_V_WIDTH)


def conv_ffn(x, w_up, conv_w, conv_b, w_down):
    h = x @ w_up
    C = h.shape[-1]
    rhs = conv_w.astype(h.dtype).reshape(CONV_W, 1, C)
    h = lax.conv_general_dilated(h, rhs, window_strides=(1,), padding=[(CONV_W - 1, 0)],
                                 dimension_numbers=('NWC', 'WIO', 'NWC'), feature_group_count=C)
    h = h + conv_b
    u, g = jnp.split(h, 2, axis=-1)
    return (jax.nn.silu(g) * u) @ w_down


def setup_inputs(seed: int = 0) -> dict:
    key = jax.random.key(seed)
    ks = jax.random.split(key, 26)
    f = jnp.float32
    L = DEPTH

    def nrm(k, shape, scale):
        return jax.random.normal(k, shape, f) * scale

    def gain(k, shape):
        return 1.0 + 0.01 * jax.random.normal(k, shape, f)

    return {
        'x': nrm(ks[0], (BATCH, SEQ, D_MODEL), 1.0),
        'p': nrm(ks[1], (DEPTH, BATCH, SEQ, PLE_DIM), 1.0),
        'attn_norm': gain(ks[2], (L, D_MODEL)),
        'w_in': nrm(ks[3], (L, D_MODEL, IN_WIDTH), D_MODEL ** -0.5),
        'cmp_k_pos': nrm(ks[4], (L, CMP_LEN, HEAD_DIM), 0.02),
        'cmp_k_w1': nrm(ks[5], (L, CMP_LEN * HEAD_DIM, CMP_HIDDEN), (CMP_LEN * HEAD_DIM) ** -0.5),
        'cmp_k_w2': nrm(ks[6], (L, CMP_HIDDEN, HEAD_DIM), CMP_HIDDEN ** -0.5),
        'cmp_v_pos': nrm(ks[7], (L, CMP_LEN, HEAD_DIM), 0.02),
        'cmp_v_w1': nrm(ks[8], (L, CMP_LEN * HEAD_DIM, CMP_HIDDEN), (CMP_LEN * HEAD_DIM) ** -0.5),
        'cmp_v_w2': nrm(ks[9], (L, CMP_HIDDEN, HEAD_DIM), CMP_HIDDEN ** -0.5),
        'nsa_out_norm': gain(ks[10], (L, NSA_WIDTH)),
        'diff_lq1': nrm(ks[11], (L, HEAD_DIM), 0.1),
        'diff_lk1': nrm(ks[12], (L, HEAD_DIM), 0.1),
        'diff_lq2': nrm(ks[13], (L, HEAD_DIM), 0.1),
        'diff_lk2': nrm(ks[14], (L, HEAD_DIM), 0.1),
        'diff_subln': gain(ks[15], (L, 2 * HEAD_DIM)),
        'w_o': nrm(ks[16], (L, MIX_WIDTH, D_MODEL), MIX_WIDTH ** -0.5),
        'ffn_norm': gain(ks[17], (L, D_MODEL)),
        'w_up': nrm(ks[18], (L, D_MODEL, 2 * D_FF), D_MODEL ** -0.5),
        'conv_w': nrm(ks[19], (L, CONV_W, 2 * D_FF), CONV_W ** -0.5),
        'conv_b': nrm(ks[20], (L, 2 * D_FF), 0.01),
        'w_down': nrm(ks[21], (L, D_FF, D_MODEL), D_FF ** -0.5),
        'ple_norm': gain(ks[22], (L, D_MODEL)),
        'w_ple_gate': nrm(ks[23], (L, D_MODEL, D_MODEL), D_MODEL ** -0.5),
        'w_ple_proj': nrm(ks[24], (L, PLE_DIM, D_MODEL), PLE_DIM ** -0.5),
        'final_norm': gain(ks[25], (D_MODEL,)),
    }


def reference(x, p, attn_norm, w_in, cmp_k_pos, cmp_k_w1, cmp_k_w2, cmp_v_pos, cmp_v_w1, cmp_v_w2,
              nsa_out_norm, diff_lq1, diff_lk1, diff_lq2, diff_lk2, diff_subln, w_o, ffn_norm,
              w_up, conv_w, conv_b, w_down, ple_norm, w_ple_gate, w_ple_proj, final_norm):
    S = x.shape[1]
    cos, sin = rope_tables(S)
    split_at = np.cumsum(IN_SPLITS)[:-1].tolist()
    h = x
    for i in range(DEPTH):
        lambda_init = 0.8 - 0.6 * math.exp(-0.3 * i)
        proj = rmsnorm(h, attn_norm[i]) @ w_in[i]
        (nq, nkc, nvc, nks, nvs, nkw, nvw, ngate, dq, dk, dv) = jnp.split(proj, split_at, axis=-1)
        y_nsa = nsa_attention(nq, nkc, nvc, nks, nvs, nkw, nvw, ngate,
                              cmp_k_pos[i], cmp_k_w1[i], cmp_k_w2[i],
                              cmp_v_pos[i], cmp_v_w1[i], cmp_v_w2[i], cos, sin)
        y_nsa = rmsnorm(y_nsa, nsa_out_norm[i])
        y_diff = diff_attention(dq, dk, dv, diff_lq1[i], diff_lk1[i], diff_lq2[i], diff_lk2[i],
                                diff_subln[i], lambda_init, cos, sin)
        h = h + jnp.concatenate([y_nsa, y_diff], axis=-1) @ w_o[i]
        h = h + conv_ffn(rmsnorm(h, ffn_norm[i]), w_up[i], conv_w[i], conv_b[i], w_down[i])
        gate = jax.nn.sigmoid(rmsnorm(h, ple_norm[i]) @ w_ple_gate[i])
        h = h + gate * (p[i] @ w_ple_proj[i])
    return rmsnorm(h, final_norm)
```

```python
import os
from contextlib import ExitStack
import numpy as np
import ml_dtypes
import concourse.bass as bass
import concourse.mybir as mybir
from concourse.bass_utils import run_bass_kernel_spmd

F32 = mybir.dt.float32
BF16 = mybir.dt.bfloat16
AF = mybir.ActivationFunctionType
ALU = mybir.AluOpType
AX = mybir.AxisListType

D = 2048
NSB = 16
NT = 9
NOWN = NT * 128
HD = 128
EPS = 1e-6
SCALE = HD ** -0.5
D_FF = 5632
NFC = D_FF // 128
LAMBDA_INIT = 0.8 - 0.6
BIGNEG = -30000.0

ENGS = ["pe", "act", "dve", "pool", "sp"]
BLK = {"pe": "tensor", "act": "scalar", "dve": "vector", "pool": "gpsimd", "sp": "sync"}


class Res:
    __slots__ = ("name", "lw", "rd")

    def __init__(self, name):
        self.name = name
        self.lw = None
        self.rd = []


class Op:
    __slots__ = ("fn", "deps", "dma", "signal")

    def __init__(self, fn, deps, dma):
        self.fn = fn
        self.deps = deps
        self.dma = dma
        self.signal = False


class Sched:
    def __init__(self):
        self.ops = {e: [] for e in ENGS}
        self.seen = {e: {} for e in ENGS}
        self.dma_cnt = {}

    def add(self, eng, fn, r=(), w=(), dma=None):
        ops = self.ops[eng]
        idx = len(ops)
        if dma is not None:
            n = self.dma_cnt.get(dma, 0) + 1
            self.dma_cnt[dma] = n
            tok = ("d", dma, n)
        else:
            tok = ("e", eng, idx)
        deps = {}
        cand = []
        for x in r:
            if x.lw is not None:
                cand.append(x.lw)
        for x in w:
            if x.lw is not None:
                cand.append(x.lw)
            cand.extend(x.rd)
        for d in cand:
            if d[0] == "e" and d[1] == eng and eng == "pe":
                continue
            key = (d[0], d[1])
            if self.seen[eng].get(key, -1) >= d[2]:
                continue
            if deps.get(key, -1) < d[2]:
                deps[key] = d[2]
        need = []
        for key, v in deps.items():
            self.seen[eng][key] = v
            need.append((key[0], key[1], v))
            if key[0] == "e":
                self.ops[key[1]][v].signal = True
        ops.append(Op(fn, need, dma))
        for x in r:
            x.rd.append(tok)
        for x in w:
            x.lw = tok
            x.rd = []
        return tok

    def barrier(self):
        toks = []
        for e in ENGS:
            if self.ops[e]:
                n = len(self.ops[e]) - 1
                if self.ops[e][n].dma is None and self.ops[e][n].fn is not None:
                    toks.append(("e", e, n))
                else:
                    k = n
                    while k >= 0 and (self.ops[e][k].dma is not None or self.ops[e][k].fn is None):
                        k -= 1
                    if k >= 0:
                        toks.append(("e", e, k))
        for key, n in self.dma_cnt.items():
            toks.append(("d", key, n))
        for e in ENGS:
            need = []
            for d in toks:
                if d[0] == "e" and d[1] == e:
                    continue
                key = (d[0], d[1])
                if self.seen[e].get(key, -1) >= d[2]:
                    continue
                self.seen[e][key] = d[2]
                need.append(d)
                if d[0] == "e":
                    self.ops[d[1]][d[2]].signal = True
            self.ops[e].append(Op(None, need, None))

    def flush(self, nc):
        self.barrier()
        if not hasattr(self, "sem_e"):
            self.sem_e = {e: nc.alloc_semaphore(name="sem_" + e) for e in ENGS}
            self.sem_d = {}
            self.pos = {e: 0 for e in ENGS}
            self.cnt = {e: 0 for e in ENGS}
        for kname in self.dma_cnt:
            if kname not in self.sem_d:
                self.sem_d[kname] = nc.alloc_semaphore(name="semd_" + kname)
        sem_e, sem_d = self.sem_e, self.sem_d
        if not hasattr(self, "val"):
            self.val = {e: [] for e in ENGS}
        for e in ENGS:
            arr = self.val[e]
            c = self.cnt[e]
            for op in self.ops[e][len(arr):]:
                if op.signal and op.dma is None:
                    c += 1
                arr.append(c)
            self.cnt[e] = c
        val = self.val
        with nc.Block() as block:
            for e in ENGS:
                deco = getattr(block, BLK[e])
                lo = self.pos[e]
                seg = self.ops[e][lo:]
                self.pos[e] = len(self.ops[e])

                def body(engine, e=e, seg=seg):
                    for op in seg:
                        for d in op.deps:
                            if d[0] == "e":
                                engine.wait_ge(sem_e[d[1]], val[d[1]][d[2]])
                            else:
                                engine.wait_ge(sem_d[d[1]], 16 * d[2])
                        if op.fn is None:
                            continue
                        ins = op.fn(engine)
                        if op.dma is not None:
                            ins.then_inc(sem_d[op.dma], 16)
                        elif op.signal:
                            ins.then_inc(sem_e[e], 1)

                deco(body)


def bc(ap, n, axis=1):
    dims = [list(x) for x in ap.ap]
    dims.insert(axis, [0, n])
    return bass.AP(ap.tensor, ap.offset, dims)


def pbc(ap, n=128):
    dims = [list(x) for x in ap.ap]
    return bass.AP(ap.tensor, ap.offset, [[0, n]] + dims[1:])


class K:
    def __init__(self, nc):
        self.nc = nc
        self.S = Sched()
        self._n = 0

    def sb(self, stack, name, shape, dt, side=None):
        self._n += 1
        if side:
            return stack.enter_context(self.nc.sbuf_tensor(f"{name}_{self._n}", list(shape), dt, side=side))
        return stack.enter_context(self.nc.sbuf_tensor(f"{name}_{self._n}", list(shape), dt))

    def din(self, name, shape, dt=F32):
        return self.nc.dram_tensor(name, list(shape), dt, kind="ExternalInput").ap()

    def dma(self, q, key, out, in_, r=(), w=(), **kw):
        return self.S.add(q, lambda e: e.dma_start(out=out, in_=in_, **kw), r, w, dma=key)

    def mm(self, out, lhsT, rhs, start, stop, r=(), w=()):
        return self.S.add("pe", lambda e: e.matmul(out, lhsT, rhs, start=start, stop=stop), r, w)

    def tr(self, out, in_, ident, r=(), w=()):
        return self.S.add("pe", lambda e: e.transpose(out, in_, ident), r, w)

    def cp(self, eng, out, in_, r=(), w=()):
        if eng == "act":
            return self.S.add("act", lambda e: e.copy(out=out, in_=in_), r, w)
        return self.S.add(eng, lambda e: e.tensor_copy(out=out, in_=in_), r, w)

    def tt(self, out, in0, in1, op, r=(), w=(), eng="dve"):
        return self.S.add(eng, lambda e: e.tensor_tensor(out=out, in0=in0, in1=in1, op=op), r, w)

    def ts(self, out, in0, s1, s2, op0, op1=None, r=(), w=(), eng="dve"):
        if op1 is None:
            return self.S.add(eng, lambda e: e.tensor_scalar(out=out, in0=in0, scalar1=s1, scalar2=None, op0=op0), r, w)
        return self.S.add(eng, lambda e: e.tensor_scalar(out=out, in0=in0, scalar1=s1, scalar2=s2, op0=op0, op1=op1), r, w)

    def stt(self, out, in0, scalar, in1, op0, op1, r=(), w=()):
        return self.S.add("dve", lambda e: e.scalar_tensor_tensor(out=out, in0=in0, scalar=scalar, in1=in1, op0=op0, op1=op1), r, w)

    def af(self, out, in_, func, r=(), w=(), **kw):
        return self.S.add("act", lambda e: e.activation(out=out, in_=in_, func=func, **kw), r, w)

    def recip(self, out, in_, r=(), w=()):
        return self.S.add("dve", lambda e: e.reciprocal(out=out, in_=in_), r, w)


def build_program(dbg=None):
    nc = bass.Bass("TRN2", target_bir_lowering=False)
    k = K(nc)
    S = k.S
    gs = ExitStack()

    class _DD:
        pass
    dd = _DD()
    _specs = {
        'xe': ("xe", [NSB * 128, D]),
        'pin': ("pin", [1024, 256]),
        'cst': ("cst", [NSB * 128, 64]),
        'validd': ("valid", [128, NSB]),
        'validcd': ("validc", [127, 1]),
        'smuld': ("smul", [128, NT, 32]),
        'saddd': ("sadd", [128, NT, 32]),
        'cmpmd': ("cmpmask", [127, NT, 128], BF16),
        'ovld': ("overlap", [127, 32]),
        'expd': ("expand", [128, NSB, 128], BF16),
        'trid': ("tri", [128, 256], BF16),
        'identd': ("ident", [128, 128], BF16),
        'gains': ("gains", [8, D]),
        'lamd': ("lamrow", [1, 512]),
        'convd': ("convw", [128, 2 * NFC, 4]),
        'w_inT': ("w_inT", [11, 128, 16, 512]),
        'w_gT': ("w_gT", [128, 16, 32]),
        'cw1T': ("cw1T", [2, 128, 32, 256]),
        'cw2T': ("cw2T", [2, 128, 2, 128]),
        'cposT': ("cposT", [128, 2, 32]),
        'w_oT': ("w_oT", [4, 128, 16, 512]),
        'w_upT': ("w_upT", [NFC, 128, 16, 256]),
        'w_dnT': ("w_dnT", [11, 4, 128, 4, 512]),
        'w_pgT': ("w_pgT", [4, 128, 16, 512]),
        'w_ppT': ("w_ppT", [4, 128, 2, 512]),
    }

    def _lazy(self, name):
        if name.startswith("_") or name not in _specs:
            raise AttributeError(name)
        sp_ = _specs[name]
        ap = k.din(*sp_)
        setattr(self, name, ap)
        return ap
    _DD.__getattr__ = _lazy
    outd = nc.dram_tensor("out", [1024, D], F32, kind="ExternalOutput").ap()
    dbgd = None
    if dbg:
        dbgd = nc.dram_tensor("dbg", list(dbg[1]), F32, kind="ExternalOutput").ap()

    def finish():
        nc._used_inputs = [_specs[n][0] for n in dd.__dict__]
        return nc

    ps = [gs.enter_context(nc.psum_tensor(f"ps{i}", [128, 512], F32)) for i in range(8)]
    psr = [Res(f"ps{i}") for i in range(8)]

    def psT(b, n):
        return ps[b][:].bitcast(BF16)[:, 0:n * 128].rearrange("p (a b) -> p a b", a=n)

    _rpad = k.sb(gs, "rpad", [128, int(os.environ.get("RPAD", "4096"))], BF16, side="right")
    if os.environ.get("RTEST"):
        _rt = k.sb(gs, "rtest", [128, int(os.environ["RTEST"])], BF16, side="right")
        S.add("dve", lambda e: e.memset(_rt[:], 1.0), [], [Res("rt")])
    ident = k.sb(gs, "ident", [128, 128], BF16)
    tri2 = k.sb(gs, "tri2", [128, 256], BF16)
    cs = k.sb(gs, "cs", [128, NSB, 64], F32)
    valid = k.sb(gs, "valid", [128, NSB], F32)
    gbh = [None]

    def alloc_gbc(stack):
        gbh[0] = k.sb(stack, "gbc", [128, D], F32)
    lam = k.sb(gs, "lam", [128, 8], F32)
    sY = ExitStack()
    yTd = k.sb(sY, "yTd", [128, 8, NOWN], BF16)
    r_yTd = [Res(f"yTd{i}") for i in range(NT)]
    wbuf = [None, None]
    r_wbuf = [Res(f"wbuf{i}") for i in range(2)]

    def alloc_wbuf(stack):
        for i in range(2):
            wbuf[i] = k.sb(stack, f"wbuf{i}", [128, 16, 512], BF16)
    r_const = Res("const")
    r_gbc = Res("gbc")
    r_lam = Res("lam")

    k.dma("sp", "c0", ident[:], dd.identd, w=[r_const])
    k.dma("sp", "c0", tri2[:], dd.trid, w=[r_const])
    k.dma("sp", "c0", cs[:], dd.cst.rearrange("(s p) c -> p s c", p=128), w=[r_const])
    k.dma("sp", "c0", valid[:], dd.validd, w=[r_const])
    tri = tri2[:, 0:128]
    anti = tri2[:, 128:256]

    def load_gain(row, width=D):
        k.dma("sp", "gbc", gbh[0][:, 0:width], pbc(dd.gains[row:row + 1, 0:width]), w=[r_gbc])

    wcnt = [0]

    def load_w(src_ap, shape_cols):
        i = wcnt[0] % 2
        wcnt[0] += 1
        kcn, ncols = shape_cols
        k.dma("pool", f"wb{i}", wbuf[i][:, 0:kcn, 0:ncols], src_ap, w=[r_wbuf[i]], max_dma_last_dim=8192)
        return wbuf[i], r_wbuf[i]

    with ExitStack() as st:
        lrow = k.sb(st, "lrow", [128, 512], F32)
        ljunk = k.sb(st, "ljunk", [128, 128], F32)
        r_lrow = Res("lrow")
        k.dma("sp", "c1", lrow[:], pbc(dd.lamd), w=[r_lrow])
        for t in range(2):
            k.tt(ljunk[:], lrow[:, 256 * t:256 * t + 128], lrow[:, 256 * t + 128:256 * t + 256], ALU.mult, r=[r_lrow], w=[r_lam])
            S.add("dve", lambda e, t=t: e.reduce_sum(out=lam[:, 1 + t:2 + t], in_=ljunk[:], axis=AX.X), [r_lam], [r_lam])
        k.af(lam[:, 3:5], lam[:, 1:3], AF.Exp, r=[r_lam], w=[r_lam])
        k.tt(lam[:, 5:6], lam[:, 4:5], lam[:, 3:4], ALU.subtract, r=[r_lam], w=[r_lam])
        k.ts(lam[:, 0:1], lam[:, 5:6], -LAMBDA_INIT, None, ALU.add, r=[r_lam], w=[r_lam])
        S.flush(nc)
    neglam = lam[:, 0:1]

    def rms_rstd(src_ap, ncols, junk, ss, r_src, r_junk, r_ss, mult=1.0):
        k.af(junk, src_ap, AF.Square, r=[r_src], w=[r_junk, r_ss], accum_out=ss[:, 0:1])
        k.af(ss[:, 2:3], ss[:, 0:1], AF.Sqrt, r=[r_ss], w=[r_ss], scale=1.0 / (ncols * mult * mult), bias=EPS / (mult * mult))
        k.recip(ss[:, 1:2], ss[:, 2:3], r=[r_ss], w=[r_ss])

    def norm_transpose(stack_bufs, src_ap, r_src, dst_of_q4, r_dst, nkc=16, pbase=0):
        xs, r_xs, sq, r_sq = stack_bufs
        rms_rstd(src_ap, nkc * 128, xs[:, 0:nkc * 128], sq, r_src, r_xs, r_sq)
        k.stt(xs[:, 0:nkc * 128], src_ap, sq[:, 1:2], gbh[0][:, 0:nkc * 128], ALU.mult, ALU.mult, r=[r_src, r_sq, r_gbc], w=[r_xs])
        for q4 in range(nkc // 4):
            pb = pbase + q4 % 2
            pT = psT(pb, 4)
            for a in range(4):
                kc = q4 * 4 + a
                k.tr(pT[:, a, :], xs[:, kc * 128:(kc + 1) * 128], ident[:], r=[r_xs, r_const], w=[psr[pb]])
            k.cp("act" if q4 % 2 == 0 else "dve", dst_of_q4(q4), pT, r=[psr[pb]], w=r_dst)

    p12 = ExitStack()
    xnT = k.sb(p12, "xnT", [128, 16, NSB * 128], BF16)
    r_xnT = [Res(f"xnT{s}") for s in range(NSB)]
    Tst = [k.sb(p12, f"Tst{i}", [128, 4, 128], BF16) for i in range(2)]
    r_T = [Res("T0"), Res("T1")]
    rtmp = [k.sb(p12, f"rtmp{i}", [128, 2, 4, 32], F32) for i in range(2)]
    alloc_wbuf(p12)

    with ExitStack() as st:
        xt = [k.sb(st, f"xt{i}", [128, D], F32) for i in range(2)]
        xs = [k.sb(st, f"xs{i}", [128, D], BF16) for i in range(2)]
        sq = [k.sb(st, f"sq{i}", [128, 4], F32) for i in range(2)]
        r_xt = [Res("xt0"), Res("xt1")]
        r_xs = [Res("xs0"), Res("xs1")]
        r_sq = [Res("sq0"), Res("sq1")]
        alloc_gbc(st)
        load_gain(0)
        for s in range(NSB):
            j = s % 2
            k.dma("sp", f"xt{j}", xt[j][:], dd.xe[s * 128:(s + 1) * 128, :], w=[r_xt[j]])
            norm_transpose((xs[j], r_xs[j], sq[j], r_sq[j]), xt[j][:], r_xt[j],
                           lambda q4, s=s: xnT[:, q4 * 4:(q4 + 1) * 4, s * 128:(s + 1) * 128], [r_xnT[s]])
        S.flush(nc)

    pcnt = [0]
    tcnt = [0]
    r_ser = [Res("ser0"), Res("ser1")]

    def project(wb, r_wb, ncols, blocks, evac):
        for sbk in blocks:
            pb = 2 + (pcnt[0] % 3)
            pcnt[0] += 1
            for kc in range(16):
                k.mm(ps[pb][:, 0:ncols], xnT[:, kc, sbk * 128:(sbk + 1) * 128], wb[:, kc, 0:ncols],
                     start=(kc == 0), stop=(kc == 15), r=[r_xnT[sbk], r_wb], w=[psr[pb]])
            evac(sbk, ps[pb], psr[pb])

    def rope_transpose(psb, r_psb, sbk, nheads, nrope, dsts):
        j = tcnt[0] % 2
        tcnt[0] += 1
        T = Tst[j]
        src = psb[:, 0:nheads * 128].rearrange("p (h d) -> p h d", h=nheads)
        if nrope < nheads:
            k.cp("act", T[:, nrope:nheads, :], src[:, nrope:nheads, :], r=[r_psb], w=[r_T[j]])
        if nrope > 0:
            cc = bc(cs[:, sbk, 0:32], nrope)
            sn = bc(cs[:, sbk, 32:64], nrope)
            tc_ = rtmp[j][:, 0, 0:nrope, :]
            ts_ = rtmp[j][:, 1, 0:nrope, :]
            k.cp("act", T[:, 0:nrope, 32:128], src[:, 0:nrope, 32:128], r=[r_psb], w=[r_T[j]])
            k.tt(tc_, src[:, 0:nrope, 0:32], cc, ALU.mult, r=[r_psb, r_const], w=[r_T[j]])
            k.tt(ts_, src[:, 0:nrope, 0:32], sn, ALU.mult, r=[r_psb, r_const], w=[r_T[j]])
            k.tt(T[:, 0:nrope, 0:16], tc_[:, :, 0:16], ts_[:, :, 16:32], ALU.subtract, r=[r_T[j]], w=[r_T[j]])
            k.tt(T[:, 0:nrope, 16:32], tc_[:, :, 16:32], ts_[:, :, 0:16], ALU.add, r=[r_T[j]], w=[r_T[j]])
        pb = tcnt[0] % 2
        pT = psT(pb, 4)
        for h in range(nheads):
            k.tr(pT[:, h, :], T[:, h, :], ident[:], r=[r_T[j], r_const], w=[psr[pb]])
        for n, (h0, hh1, dst, r_dst) in enumerate(dsts):
            k.cp("act" if n == 0 else "dve", dst, pT[:, h0:hh1, :], r=[psr[pb]], w=list(r_dst) + [r_ser[pb]])

    own_blocks = list(range(7, 16))
    all_blocks = list(range(NSB))

    with ExitStack() as sA:
        dKT = k.sb(sA, "dKT", [128, 4, NSB * 128], BF16)
        dVa = k.sb(sA, "dVa", [128, NSB, 2, 257], BF16)
        dQT = k.sb(sA, "dQT", [128, 4, NOWN], BF16)
        r_dKT = [Res(f"dKT{s}") for s in range(NSB)]
        r_dVa = [Res(f"dVa{s}") for s in range(NSB)]
        r_dQT = [Res(f"dQT{i}") for i in range(NT)]
        Eb = [k.sb(sA, f"E{i}", [128, 384], BF16) for i in range(3)]
        r_E = [Res(f"E{i}") for i in range(3)]
        o1 = [k.sb(sA, f"o1_{i}", [128, 256], F32) for i in range(3)]
        r_o1 = [Res(f"o1_{i}") for i in range(3)]
        od = [k.sb(sA, f"od{i}", [128, 256], F32) for i in range(2)]
        r_od = [Res("od0"), Res("od1")]
        ydn = [k.sb(sA, f"ydn{i}", [128, 256], BF16) for i in range(2)]
        r_ydn = [Res("ydn0"), Res("ydn1")]
        rr = [k.sb(sA, f"rr{i}", [128, 8], F32) for i in range(2)]
        r_rr = [Res("rr0"), Res("rr1")]
        ojunk = k.sb(sA, "ojunk", [128, 256], BF16)
        r_ojunk = Res("ojunk")
        alloc_gbc(sA)
        load_gain(5, 256)
        ecnt = 0
        fcnt = 0
        scnt = 0
        for hp in range(2):
            wb, r_wb = load_w(dd.w_inT[3 * hp + 0], (16, 512))

            def ev(sbk, psb, r_psb):
                i = sbk - 7
                rope_transpose(psb, r_psb, sbk, 4, 4, [(0, 4, dQT[:, :, i * 128:(i + 1) * 128], [r_dQT[i]])])
            project(wb, r_wb, 512, own_blocks, ev)
            wb, r_wb = load_w(dd.w_inT[3 * hp + 1], (16, 512))

            def ev(sbk, psb, r_psb):
                rope_transpose(psb, r_psb, sbk, 4, 4, [(0, 4, dKT[:, :, sbk * 128:(sbk + 1) * 128], [r_dKT[sbk]])])
            project(wb, r_wb, 512, all_blocks, ev)
            wb, r_wb = load_w(dd.w_inT[3 * hp + 2], (16, 512))

            def ev(sbk, psb, r_psb):
                src = psb[:, 0:512].rearrange("p (h d) -> p h d", h=2)
                k.cp("act", dVa[:, sbk, :, 0:256], src, r=[r_psb], w=[r_dVa[sbk]])
                k.cp("dve", dVa[:, sbk, :, 256:257], bc(valid[:, sbk:sbk + 1], 2), r=[r_const], w=[r_dVa[sbk]])
            project(wb, r_wb, 512, all_blocks, ev)

            for hl in range(2):
                h = 2 * hp + hl
                for g in range(3):
                    lo = 3 * g
                    for m in range(2):
                        hm = 2 * hl + m
                        for s in range(7 + lo + 3):
                            i0 = max(lo, s - 7)
                            nq = (lo + 3 - i0) * 128
                            sp_ = scnt % 2
                            scnt += 1
                            k.mm(ps[sp_][:, 0:nq], dKT[:, hm, s * 128:(s + 1) * 128], dQT[:, hm, i0 * 128:(lo + 3) * 128],
                                 start=True, stop=True, r=[r_dKT[s]] + [r_dQT[i] for i in range(i0, lo + 3)], w=[psr[sp_]])
                            ej = ecnt % 3
                            ecnt += 1
                            E = Eb[ej]
                            k.af(E[:, 0:nq], ps[sp_][:, 0:nq], AF.Exp, r=[psr[sp_]], w=[r_E[ej]], scale=SCALE)
                            if s - 7 >= lo:
                                k.tt(E[:, 0:128], E[:, 0:128], tri, ALU.mult, r=[r_E[ej], r_const], w=[r_E[ej]])
                            for i in range(i0, lo + 3):
                                a = i - lo
                                acc = ps[2 + a]
                                r_acc = psr[2 + a]
                                k.mm(acc[:, 0:257], E[:, (i - i0) * 128:(i - i0 + 1) * 128], dVa[:, s, hl, :],
                                     start=(s == 0), stop=(s == 7 + i), r=[r_E[ej], r_dVa[s]], w=[r_acc])
                                if s != 7 + i:
                                    continue
                                fj = fcnt % 2
                                if m == 0:
                                    k.ts(rr[fj][:, 5:6], acc[:, 256:257], 1e-30, None, ALU.max, r=[r_acc], w=[r_rr[fj]])
                                    k.recip(rr[fj][:, 0:1], rr[fj][:, 5:6], r=[r_rr[fj]], w=[r_rr[fj]])
                                    k.ts(o1[a][:], acc[:, 0:256], rr[fj][:, 0:1], None, ALU.mult, r=[r_acc, r_rr[fj]], w=[r_o1[a]])
                                    continue
                                fcnt += 1
                                k.ts(rr[fj][:, 5:6], acc[:, 256:257], 1e-30, None, ALU.max, r=[r_acc], w=[r_rr[fj]])
                                k.recip(rr[fj][:, 0:1], rr[fj][:, 5:6], r=[r_rr[fj]], w=[r_rr[fj]])
                                k.tt(rr[fj][:, 1:2], rr[fj][:, 0:1], neglam, ALU.mult, r=[r_rr[fj], r_lam], w=[r_rr[fj]])
                                k.stt(od[fj][:], acc[:, 0:256], rr[fj][:, 1:2], o1[a][:], ALU.mult, ALU.add,
                                      r=[r_acc, r_rr[fj], r_o1[a]], w=[r_od[fj]])
                                if dbg and dbg[0] == "ydiff":
                                    k.dma("sp", "dbg", dbgd[i * 128:(i + 1) * 128, h * 256:(h + 1) * 256], od[fj][:], r=[r_od[fj]])
                                k.af(ojunk[:], od[fj][:], AF.Square, r=[r_od[fj]], w=[r_ojunk, r_rr[fj]], accum_out=rr[fj][:, 2:3])
                                mlt = 1.0 - LAMBDA_INIT
                                k.af(rr[fj][:, 3:4], rr[fj][:, 2:3], AF.Sqrt, r=[r_rr[fj]], w=[r_rr[fj]],
                                     scale=1.0 / (256 * mlt * mlt), bias=EPS / (mlt * mlt))
                                k.recip(rr[fj][:, 4:5], rr[fj][:, 3:4], r=[r_rr[fj]], w=[r_rr[fj]])
                                k.stt(ydn[fj][:], od[fj][:], rr[fj][:, 4:5], gbh[0][:, 0:256], ALU.mult, ALU.mult,
                                      r=[r_od[fj], r_rr[fj], r_gbc], w=[r_ydn[fj]])
                                pT = psT(5 + fj, 2)
                                for c2 in range(2):
                                    k.tr(pT[:, c2, :], ydn[fj][:, c2 * 128:(c2 + 1) * 128], ident[:], r=[r_ydn[fj], r_const], w=[psr[5 + fj]])
                                k.cp("act", yTd[:, 2 * h:2 * h + 2, i * 128:(i + 1) * 128], pT, r=[psr[5 + fj]], w=[r_yTd[i]])
        S.flush(nc)

    if dbg and dbg[0] == "ydiff":
        return finish()


    sBp = ExitStack()
    KnT = k.sb(sBp, "KnT", [128, 4, 2, NSB * 128], BF16, side="right")
    Vau = k.sb(sBp, "Vau", [128, NSB, 2, 2, 129], BF16, side="right")
    vcT = KnT[:, 3]
    QnT = k.sb(sBp, "QnT", [128, NT, 2, 512], BF16, side="right")
    gate = k.sb(sBp, "gate", [128, NT, 24], F32, side="right")
    r_KnT = [Res(f"KnT{s}") for s in range(NSB)]
    r_Vau = [Res(f"Vau{s}") for s in range(NSB)]
    r_vcT = [Res(f"vcT{s}") for s in range(NSB)]
    r_QnT = [Res(f"QnT{i}") for i in range(NT)]
    r_gate = [Res(f"gate{i}") for i in range(NT)]

    for kv in range(2):
        wb, r_wb = load_w(dd.w_inT[6 + kv], (16, 512))

        def ev(sbk, psb, r_psb, kv=kv):
            i = sbk - 7
            rope_transpose(psb, r_psb, sbk, 4, 4,
                           [(0, 4, QnT[:, i, kv, :].rearrange("p (g q) -> p g q", g=4), [r_QnT[i]])])
        project(wb, r_wb, 512, own_blocks, ev)
    if dbg and dbg[0] == "b1":
        S.flush(nc)
        return finish()
    wb, r_wb = load_w(dd.w_inT[8], (16, 512))

    def ev(sbk, psb, r_psb):
        sl = slice(sbk * 128, (sbk + 1) * 128)
        rope_transpose(psb, r_psb, sbk, 4, 4, [(0, 2, KnT[:, 0, :, sl], [r_KnT[sbk]]), (2, 4, KnT[:, 1, :, sl], [r_KnT[sbk]])])
    project(wb, r_wb, 512, all_blocks, ev)
    if dbg and dbg[0] == "b2":
        S.flush(nc)
        return finish()
    wb, r_wb = load_w(dd.w_inT[9], (16, 512))

    def ev(sbk, psb, r_psb):
        sl = slice(sbk * 128, (sbk + 1) * 128)
        rope_transpose(psb, r_psb, sbk, 4, 2, [(0, 2, KnT[:, 2, :, sl], [r_KnT[sbk]]), (2, 4, vcT[:, :, sl], [r_vcT[sbk]])])
    project(wb, r_wb, 512, all_blocks, ev)
    if dbg and dbg[0] == "b3":
        S.flush(nc)
        return finish()
    wb, r_wb = load_w(dd.w_inT[10], (16, 512))

    def ev(sbk, psb, r_psb):
        src = psb[:, 0:512].rearrange("p (b h d) -> p b h d", b=2, h=2)
        for br in range(2):
            k.cp("act" if br == 0 else "dve", Vau[:, sbk, br, :, 0:128], src[:, br], r=[r_psb], w=[r_Vau[sbk]])
            k.cp("dve", Vau[:, sbk, br, :, 128:129], bc(valid[:, sbk:sbk + 1], 2), r=[r_const], w=[r_Vau[sbk]])
    project(wb, r_wb, 512, all_blocks, ev)
    if dbg and dbg[0] == "b4":
        S.flush(nc)
        return finish()
    wb, r_wb = load_w(dd.w_gT, (16, 32))

    def ev(sbk, psb, r_psb):
        i = sbk - 7
        k.af(gate[:, i, :], psb[:, 0:24], AF.Sigmoid, r=[r_psb], w=[r_gate[i]])
    project(wb, r_wb, 32, own_blocks, ev)
    S.flush(nc)
    if dbg and dbg[0] == "bproj":
        return finish()
    p12.close()

    sB2 = ExitStack()
    yTn = k.sb(sB2, "yTn", [128, 8, NOWN], BF16)
    r_yTn = [Res(f"yTn{i}") for i in range(NT)]
    with ExitStack() as st:
        cmpm = k.sb(st, "cmpm", [128, NT, 128], BF16)
        ovl = k.sb(st, "ovl", [128, 32], F32)
        expn = k.sb(st, "expn", [128, NSB, 128], BF16)
        smul = k.sb(st, "smul", [128, NT, 32], F32)
        sadd = k.sb(st, "sadd", [128, NT, 32], F32)
        vldc = k.sb(st, "vldc", [128, 1], F32)
        posT = k.sb(st, "posT", [128, 2, 32], F32)
        KcT = k.sb(st, "KcT", [128, 2, 128], BF16)
        Rc = k.sb(st, "Rc", [128, 2, 161], BF16)
        r_c2 = Res("c2")
        r_KcT = Res("KcT")
        r_Rc = Res("Rc")
        k.dma("sp", "c2", cmpm[0:127], dd.cmpmd, w=[r_c2])
        k.dma("sp", "c2", ovl[0:127], dd.ovld, w=[r_c2])
        k.dma("sp", "c2", expn[:], dd.expd, w=[r_c2])
        k.dma("sp", "c2", smul[:], dd.smuld, w=[r_c2])
        k.dma("sp", "c2", sadd[:], dd.saddd, w=[r_c2])
        k.dma("sp", "c2", vldc[0:127], dd.validcd, w=[r_c2])
        k.dma("sp", "c2", posT[:], dd.cposT, w=[r_c2])

        with ExitStack() as st2:
            w1 = k.sb(st2, "w1", [128, 32, 256], BF16)
            w2 = k.sb(st2, "w2", [128, 2, 128], BF16)
            A2 = [k.sb(st2, f"A2{ab}", [128, 16, 128], BF16) for ab in range(2)]
            hs = k.sb(st2, "hs", [128, 128], F32)
            uu = k.sb(st2, "uu", [128, 128], F32)
            hT = k.sb(st2, "hT", [128, 2, 128], BF16)
            r_w1, r_w2, r_hs, r_uu, r_hT = Res("w1"), Res("w2"), Res("hs"), Res("uu"), Res("hT")
            r_A = [Res("A20"), Res("A21")]
            for t in range(2):
                k.dma("pool", "w1", w1[:], dd.cw1T[t], w=[r_w1], max_dma_last_dim=8192)
                k.dma("pool", "w2", w2[:], dd.cw2T[t], w=[r_w2])
                for kv in range(2):
                    if t == 0:
                        src = KnT[:, 0, kv, :].rearrange("p (g l) -> p g l", l=16)
                        r_src = r_KnT
                    else:
                        src = vcT[:, kv, :].rearrange("p (g l) -> p g l", l=16)
                        r_src = r_vcT
                    for ab in range(2):
                        k.tt(A2[ab][:].rearrange("p l g -> p g l"), src, bc(posT[:, t, 16 * ab:16 * ab + 16], 128, axis=1), ALU.add,
                             r=list(r_src) + [r_c2], w=[r_A[ab]])
                    for nch in range(2):
                        pb = 6 + nch
                        for l in range(32):
                            rhs = A2[0][:, l, 0:127] if l < 16 else A2[1][:, l - 16, 1:128]
                            k.mm(ps[pb][:, 0:127], w1[:, l, nch * 128:(nch + 1) * 128], rhs, start=(l == 0), stop=(l == 31),
                                 r=[r_w1, r_A[0], r_A[1]], w=[psr[pb]])
                        k.cp("act", hs[:, 0:127], ps[pb][:, 0:127], r=[psr[pb]], w=[r_hs])
                        k.tt(uu[:, 0:127], hs[:, 0:127], hs[:, 0:127], ALU.mult, r=[r_hs], w=[r_uu])
                        k.ts(uu[:, 0:127], uu[:, 0:127], 0.044715, 1.0, ALU.mult, ALU.add, r=[r_uu], w=[r_uu])
                        k.tt(uu[:, 0:127], uu[:, 0:127], hs[:, 0:127], ALU.mult, r=[r_uu, r_hs], w=[r_uu])
                        k.af(uu[:, 0:127], uu[:, 0:127], AF.Sigmoid, r=[r_uu], w=[r_uu], scale=1.5957691216057308)
                        k.tt(hT[:, nch, 0:127], uu[:, 0:127], hs[:, 0:127], ALU.mult, r=[r_uu, r_hs], w=[r_hT])
                    pb = 5
                    if t == 0:
                        for nch in range(2):
                            k.mm(ps[pb][:, 0:127], w2[:, nch, :], hT[:, nch, 0:127], start=(nch == 0), stop=(nch == 1),
                                 r=[r_w2, r_hT], w=[psr[pb]])
                        k.cp("act", KcT[:, kv, 0:127], ps[pb][:, 0:127], r=[psr[pb]], w=[r_KcT])
                    else:
                        for nch in range(2):
                            k.mm(ps[pb][0:127, 0:128], hT[:, nch, 0:127], w2[:, nch, :], start=(nch == 0), stop=(nch == 1),
                                 r=[r_w2, r_hT], w=[psr[pb]])
                        k.ts(Rc[0:127, kv, 0:128], ps[pb][0:127, 0:128], vldc[0:127, 0:1], None, ALU.mult, r=[psr[pb], r_c2], w=[r_Rc])
                        k.cp("dve", Rc[0:127, kv, 128:129], vldc[0:127, 0:1], r=[r_c2], w=[r_Rc])
                        k.ts(Rc[0:127, kv, 129:161], ovl[0:127, :], vldc[0:127, 0:1], None, ALU.mult, r=[r_c2], w=[r_Rc])
            S.flush(nc)
            if dbg and dbg[0] == "cmp":
                return finish()

        with ExitStack() as st2:
            Eb = [k.sb(st2, f"En{i}", [128, 512], BF16) for i in range(3)]
            r_E = [Res(f"En{i}") for i in range(3)]
            onsa = [k.sb(st2, f"onsa{i}", [128, 8, 128], F32) for i in range(2)]
            r_onsa = [Res("onsa0"), Res("onsa1")]
            scl = k.sb(st2, "scl", [128, 16], F32)
            psl = k.sb(st2, "psl", [128, 32], F32)
            scr = k.sb(st2, "scr", [128, 32], F32)
            sc2 = k.sb(st2, "sc2", [128, 32], F32)
            m8 = k.sb(st2, "m8", [128, 16], F32)
            selb = k.sb(st2, "selb", [128, 128], BF16)
            selT = k.sb(st2, "selT", [128, 4, 128], BF16)
            xsn = k.sb(st2, "xsn", [128, 1024], BF16)
            sqn = k.sb(st2, "sqn", [128, 4], F32)
            r_scl, r_sel, r_selT, r_xsn, r_sqn = Res("scl"), Res("sel"), Res("selT"), Res("xsn"), Res("sqn")
            alloc_gbc(st2)
            load_gain(4, 1024)
            S.add("dve", lambda e: e.memset(selb[:], 0.0), [], [r_sel])
            S.add("dve", lambda e: e.memset(selT[:], 0.0), [], [r_selT])
            cnt = {"s": 0, "e": 0}

            def next_s():
                cnt["s"] += 1
                return cnt["s"] % 2

            def next_e():
                cnt["e"] += 1
                return cnt["e"] % 3

            def fin_scalars(lcols, i, kv, gbr):
                for g, (ap_l, r_l) in enumerate(lcols):
                    k.ts(scl[:, g:g + 1], ap_l, 1e-30, None, ALU.max, r=[r_l], w=[r_scl])
                k.recip(scl[:, 4:8], scl[:, 0:4], r=[r_scl], w=[r_scl])
                k.tt(scl[:, 8:12], scl[:, 4:8], gate[:, i, gbr * 8 + kv * 4:gbr * 8 + kv * 4 + 4], ALU.mult, r=[r_scl, r_gate[i]], w=[r_scl])

            def branch(i, kv, oj, slots, kbr, vbr, gbr, first_anti, use_bias):
                Q = QnT[:, i, kv, :]
                for n, s in enumerate(slots):
                    sp_ = next_s()
                    k.mm(ps[sp_][:, :], KnT[:, kbr, kv, s * 128:(s + 1) * 128], Q, start=True, stop=not use_bias,
                         r=[r_KnT[s], r_QnT[i]], w=[psr[sp_]])
                    if use_bias:
                        k.mm(ps[sp_][:, :], expn[:, s, :], selT[:].rearrange("p g q -> p (g q)"), start=False, stop=True,
                             r=[r_c2, r_selT], w=[psr[sp_]])
                    ej = next_e()
                    E = Eb[ej]
                    k.af(E[:, :], ps[sp_][:, :], AF.Exp, r=[psr[sp_]], w=[r_E[ej]], scale=SCALE)
                    msk = tri if s == 7 + i else (anti if (first_anti and n == 0) else None)
                    if msk is not None:
                        E4 = E[:, :].rearrange("p (g q) -> p g q", g=4)
                        k.tt(E4, E4, bc(msk, 4), ALU.mult, r=[r_E[ej], r_const], w=[r_E[ej]])
                    for g in range(4):
                        k.mm(ps[2 + g][:, 0:129], E[:, g * 128:(g + 1) * 128], Vau[:, s, vbr, kv, :], start=(n == 0),
                             stop=(n == len(slots) - 1), r=[r_E[ej], r_Vau[s]], w=[psr[2 + g]])
                fin_scalars([(ps[2 + g][:, 128:129], psr[2 + g]) for g in range(4)], i, kv, gbr)
                for g in range(4):
                    o_ = onsa[oj][:, kv * 4 + g, :]
                    k.stt(o_, ps[2 + g][:, 0:128], scl[:, 8 + g:9 + g], o_, ALU.mult, ALU.add, r=[psr[2 + g], r_scl], w=[r_onsa[oj]])

            for i in range(NT):
                oj = i % 2
                for kv in range(2):
                    Q = QnT[:, i, kv, :]
                    sp_ = next_s()
                    k.mm(ps[sp_][0:127, :], KcT[:, kv, 0:127], Q, start=True, stop=True, r=[r_KcT, r_QnT[i]], w=[psr[sp_]])
                    ej = next_e()
                    E = Eb[ej]
                    k.af(E[0:127, :], ps[sp_][0:127, :], AF.Exp, r=[psr[sp_]], w=[r_E[ej]], scale=SCALE)
                    E4 = E[0:127, :].rearrange("p (g q) -> p g q", g=4)
                    k.tt(E4, E4, bc(cmpm[0:127, i, :], 4), ALU.mult, r=[r_E[ej], r_c2], w=[r_E[ej]])
                    C = []
                    for g in range(4):
                        cb = 6 + g // 2
                        cap = ps[cb][:, (g % 2) * 161:(g % 2) * 161 + 161]
                        k.mm(cap, E[0:127, g * 128:(g + 1) * 128], Rc[0:127, kv, :], start=True, stop=True, r=[r_E[ej], r_Rc], w=[psr[cb]])
                        C.append((cap, psr[cb]))
                    fin_scalars([(C[g][0][:, 128:129], C[g][1]) for g in range(4)], i, kv, 0)
                    for g in range(4):
                        k.ts(onsa[oj][:, kv * 4 + g, :], C[g][0][:, 0:128], scl[:, 8 + g:9 + g], None, ALU.mult,
                             r=[C[g][1], r_scl], w=[r_onsa[oj]])
                    k.ts(psl[:], C[0][0][:, 129:161], scl[:, 4:5], None, ALU.mult, r=[C[0][1], r_scl], w=[r_sel])
                    for g in range(1, 4):
                        k.stt(psl[:], C[g][0][:, 129:161], scl[:, 4 + g:5 + g], psl[:], ALU.mult, ALU.add, r=[C[g][1], r_scl, r_sel], w=[r_sel])
                    k.tt(scr[:], psl[:], smul[:, i, :], ALU.mult, r=[r_sel, r_c2], w=[r_sel])
                    k.tt(scr[:], scr[:], sadd[:, i, :], ALU.add, r=[r_sel, r_c2], w=[r_sel])
                    S.add("dve", lambda e: e.max(out=m8[:, 0:8], in_=scr[:]), [r_sel], [r_sel])
                    S.add("dve", lambda e: e.match_replace(out=sc2[:], in_to_replace=m8[:, 0:8], in_values=scr[:], imm_value=-1e9),
                          [r_sel], [r_sel])
                    S.add("dve", lambda e: e.max(out=m8[:, 8:16], in_=sc2[:]), [r_sel], [r_sel])
                    k.ts(selb[:, 0:32], scr[:], m8[:, 15:16], BIGNEG, ALU.is_lt, ALU.mult, r=[r_sel], w=[r_sel])
                    tb = next_s()
                    pTs = ps[tb][:].bitcast(BF16)[:, 0:128]
                    k.tr(pTs, selb[:], ident[:], r=[r_sel, r_const], w=[psr[tb]])
                    k.cp("act", selT[0:32], bc(pTs[0:32, :], 4), r=[psr[tb]], w=[r_selT])
                    branch(i, kv, oj, list(range(3 + i, 8 + i)), 2, 1, 2, True, False)
                    branch(i, kv, oj, list(range(0, 8 + i)), 1, 0, 1, False, True)
                norm_transpose((xsn, r_xsn, sqn, r_sqn), onsa[oj][:].rearrange("p h d -> p (h d)"), r_onsa[oj],
                               lambda q4, i=i: yTn[:, q4 * 4:(q4 + 1) * 4, i * 128:(i + 1) * 128], [r_yTn[i]], nkc=8, pbase=6)
                if dbg and dbg[0] == "nsa1" and i == 0:
                    break
            S.flush(nc)
            if dbg and dbg[0] in ("nsa", "nsa1"):
                return finish()
    sBp.close()

    sH = ExitStack()
    h1 = k.sb(sH, "h1", [128, NT, D], F32, side="right")
    r_h1 = [Res(f"h1_{i}") for i in range(NT)]
    r_h1all = Res("h1all")
    with ExitStack() as st:
        alloc_wbuf(st)
        for i in range(NT):
            k.dma("sp", "h1l", h1[:, i, :], dd.xe[(7 + i) * 128:(8 + i) * 128, :], w=[r_h1[i], r_h1all])
        for c4 in range(4):
            wb, r_wb = load_w(dd.w_oT[c4], (16, 512))
            for i in range(NT):
                pb = 2 + (pcnt[0] % 3)
                pcnt[0] += 1
                for kc in range(16):
                    lhsT = yTn[:, kc, i * 128:(i + 1) * 128] if kc < 8 else yTd[:, kc - 8, i * 128:(i + 1) * 128]
                    k.mm(ps[pb][:, :], lhsT, wb[:, kc, :], start=(kc == 0), stop=(kc == 15), r=[r_yTn[i], r_yTd[i], r_wb], w=[psr[pb]])
                hv = h1[:, i, c4 * 512:(c4 + 1) * 512]
                k.tt(hv, ps[pb][:, :], hv, ALU.add, r=[psr[pb], r_h1all, r_h1[i]], w=[r_h1[i]])
        S.flush(nc)
    sB2.close()
    sY.close()
    if dbg and dbg[0] in ("hh", "h1"):
        k.dma("sp", "dbg", dbgd[0].rearrange("(i p) c -> p i c", p=128), h1[:], r=r_h1)
        S.flush(nc)
        if dbg[0] == "h1":
            return finish()

    with ExitStack() as sF:
        xn2T = k.sb(sF, "xn2T", [128, 16, NOWN], BF16)
        r_xn2T = [Res(f"xn2T{i}") for i in range(NT)]
        _xs2 = k.sb(sF, "xs2", [128, D], BF16)
        xs2 = [_xs2, _xs2]
        sq2 = [k.sb(sF, f"sq2{j}", [128, 4], F32) for j in range(2)]
        _rxs2 = Res("xs2")
        r_xs2 = [_rxs2, _rxs2]
        r_sq2 = [Res("sq20"), Res("sq21")]
        cvw = k.sb(sF, "cvw", [128, 2 * NFC, 4], F32)
        r_c3 = Res("c3")
        k.dma("sp", "c3", cvw[:], dd.convd, w=[r_c3])
        alloc_gbc(sF)
        load_gain(1)
        for i in range(NT):
            j = i % 2
            norm_transpose((xs2[j], r_xs2[j], sq2[j], r_sq2[j]), h1[:, i, :], r_h1[i],
                           lambda q4, i=i: xn2T[:, q4 * 4:(q4 + 1) * 4, i * 128:(i + 1) * 128], [r_xn2T[i]], pbase=0)
        raw = [[k.sb(sF, f"raw{j}{t}", [128, 2 + NOWN], F32) for t in range(2)] for j in range(2)]
        r_raw = [[Res(f"raw{j}{t}") for t in range(2)] for j in range(2)]
        yv = [k.sb(sF, f"yv{t}", [128, NOWN], F32) for t in range(2)]
        r_yv = [Res("yv0"), Res("yv1")]
        aT = [k.sb(sF, f"aT{j}", [128, 4, 1024], BF16) for j in range(2)]
        r_aT = [Res("aT0"), Res("aT1")]
        wub = [k.sb(sF, f"wub{j}", [128, 16, 256], BF16) for j in range(2)]
        r_wub = [Res("wub0"), Res("wub1")]
        wdb = [k.sb(sF, f"wdb{j}", [128, 4, 512], BF16) for j in range(3)]
        r_wdb = [Res(f"wdb{j}") for j in range(3)]
        for j in range(2):
            for t in range(2):
                S.add("dve", lambda e, j=j, t=t: e.memset(raw[j][t][:, 0:2], 0.0), [], [r_raw[j][t]])
        TG = [(0, 512), (512, 512), (1024, 128)]
        ucnt = 0
        dcnt = 0
        qcnt = 0
        for gi in range(11):
            aj = gi % 2
            for cc in range(4):
                fc = gi * 4 + cc
                wj = fc % 2
                k.dma("pool", f"wu{wj}", wub[wj][:], dd.w_upT[fc], w=[r_wub[wj]], max_dma_last_dim=8192)
                for t in range(2):
                    for (t0, nt) in TG:
                        pb = 2 + (ucnt % 4)
                        ucnt += 1
                        tl = list(range(t0 // 128, (t0 + nt) // 128))
                        for kc in range(16):
                            k.mm(ps[pb][:, 0:nt], wub[wj][:, kc, t * 128:(t + 1) * 128], xn2T[:, kc, t0:t0 + nt], start=(kc == 0), stop=(kc == 15),
                                 r=[r_wub[wj]] + [r_xn2T[q] for q in tl], w=[psr[pb]])
                        k.cp("act", raw[wj][t][:, 2 + t0:2 + t0 + nt], ps[pb][:, 0:nt], r=[psr[pb]], w=[r_raw[wj][t]])
                    ch = fc + t * NFC
                    k.ts(yv[t][:], raw[wj][t][:, 2:2 + NOWN], cvw[:, ch, 2:3], cvw[:, ch, 3:4], ALU.mult, ALU.add,
                         r=[r_raw[wj][t], r_c3], w=[r_yv[t]])
                    k.stt(yv[t][:], raw[wj][t][:, 1:1 + NOWN], cvw[:, ch, 1:2], yv[t][:], ALU.mult, ALU.add, r=[r_raw[wj][t], r_c3, r_yv[t]], w=[r_yv[t]])
                    k.stt(yv[t][:], raw[wj][t][:, 0:NOWN], cvw[:, ch, 0:1], yv[t][:], ALU.mult, ALU.add, r=[r_raw[wj][t], r_c3, r_yv[t]], w=[r_yv[t]])
                k.af(yv[1][:, 128:NOWN], yv[1][:, 128:NOWN], AF.Silu, r=[r_yv[1]], w=[r_yv[1]])
                k.tt(aT[aj][:, cc, :], yv[1][:, 128:NOWN], yv[0][:, 128:NOWN], ALU.mult, r=[r_yv[0], r_yv[1]], w=[r_aT[aj]])
            for c4 in range(4):
                dj = dcnt % 3
                dcnt += 1
                k.dma("pool", f"wd{dj}", wdb[dj][:], dd.w_dnT[gi, c4], w=[r_wdb[dj]], max_dma_last_dim=8192)
                for i in range(1, NT):
                    pb = 6 + (qcnt % 2)
                    qcnt += 1
                    for cc in range(4):
                        k.mm(ps[pb][:, :], aT[aj][:, cc, (i - 1) * 128:i * 128], wdb[dj][:, cc, :], start=(cc == 0), stop=(cc == 3),
                             r=[r_aT[aj], r_wdb[dj]], w=[psr[pb]])
                    hv = h1[:, i, c4 * 512:(c4 + 1) * 512]
                    k.tt(hv, ps[pb][:, :], hv, ALU.add, r=[psr[pb], r_h1[i]], w=[r_h1[i]])
        S.flush(nc)
    if dbg and dbg[0] == "hh":
        k.dma("sp", "dbg", dbgd[1].rearrange("(i p) c -> p i c", p=128), h1[:], r=r_h1)
        S.flush(nc)

    with ExitStack() as sP:
        alloc_wbuf(sP)
        xn3T = k.sb(sP, "xn3T", [128, 16, 1024], BF16)
        r_xn3T = [Res(f"xn3T{i}") for i in range(8)]
        ppT = k.sb(sP, "ppT", [128, 2, 1024], BF16)
        r_ppT = [Res(f"ppT{i}") for i in range(8)]
        pf = k.sb(sP, "pf", [128, 8, 256], F32)
        pb16 = k.sb(sP, "pb16", [128, 8, 256], BF16)
        r_pf, r_pb16 = Res("pf"), Res("pb16")
        xs3 = [k.sb(sP, f"xs3{j}", [128, D], BF16) for j in range(2)]
        sq3 = [k.sb(sP, f"sq3{j}", [128, 4], F32) for j in range(2)]
        r_xs3 = [Res("xs30"), Res("xs31")]
        r_sq3 = [Res("sq30"), Res("sq31")]
        wpp = [k.sb(sP, f"wpp{j}", [128, 2, 512], BF16) for j in range(2)]
        r_wpp = [Res("wpp0"), Res("wpp1")]
        gsg = [k.sb(sP, f"gsg{j}", [128, 512], F32) for j in range(2)]
        r_gsg = [Res("gsg0"), Res("gsg1")]
        _outt = k.sb(sP, "outt", [128, D], F32)
        _routt = Res("outt")
        outt = [_outt, _outt]
        r_outt = [_routt, _routt]
        k.dma("sp", "pf", pf[:], dd.pin.rearrange("(i p) c -> p i c", p=128), w=[r_pf])
        k.cp("dve", pb16[:], pf[:], r=[r_pf], w=[r_pb16])
        alloc_gbc(sP)
        load_gain(2)
        for i in range(1, NT):
            j = i % 2
            norm_transpose((xs3[j], r_xs3[j], sq3[j], r_sq3[j]), h1[:, i, :], r_h1[i],
                           lambda q4, i=i: xn3T[:, q4 * 4:(q4 + 1) * 4, (i - 1) * 128:i * 128], [r_xn3T[i - 1]], pbase=0)
            pT = psT(6 + j, 2)
            for c2 in range(2):
                k.tr(pT[:, c2, :], pb16[:, i - 1, c2 * 128:(c2 + 1) * 128], ident[:], r=[r_pb16, r_const], w=[psr[6 + j]])
            k.cp("act", ppT[:, :, (i - 1) * 128:i * 128], pT, r=[psr[6 + j]], w=[r_ppT[i - 1]])
        gcnt = 0
        for c4 in range(4):
            wb, r_wb = load_w(dd.w_pgT[c4], (16, 512))
            wj = c4 % 2
            k.dma("pool", f"wp{wj}", wpp[wj][:], dd.w_ppT[c4], w=[r_wpp[wj]], max_dma_last_dim=8192)
            for i in range(1, NT):
                j = gcnt % 2
                gcnt += 1
                pa, pq = 2 + j, 4 + j
                tk = slice((i - 1) * 128, i * 128)
                for kc in range(16):
                    k.mm(ps[pa][:, :], xn3T[:, kc, tk], wb[:, kc, :], start=(kc == 0), stop=(kc == 15), r=[r_xn3T[i - 1], r_wb], w=[psr[pa]])
                for kc in range(2):
                    k.mm(ps[pq][:, :], ppT[:, kc, tk], wpp[wj][:, kc, :], start=(kc == 0), stop=(kc == 1), r=[r_ppT[i - 1], r_wpp[wj]], w=[psr[pq]])
                k.af(gsg[j][:], ps[pa][:, :], AF.Sigmoid, r=[psr[pa]], w=[r_gsg[j]])
                k.tt(gsg[j][:], gsg[j][:], ps[pq][:, :], ALU.mult, r=[r_gsg[j], psr[pq]], w=[r_gsg[j]])
                hv = h1[:, i, c4 * 512:(c4 + 1) * 512]
                k.tt(hv, hv, gsg[j][:], ALU.add, r=[r_gsg[j], r_h1[i]], w=[r_h1[i]])
        load_gain(3)
        for i in range(1, NT):
            j = i % 2
            rms_rstd(h1[:, i, :], D, xs3[j][:], sq3[j], r_h1[i], r_xs3[j], r_sq3[j])
            k.stt(outt[j][:], h1[:, i, :], sq3[j][:, 1:2], gbh[0][:], ALU.mult, ALU.mult, r=[r_h1[i], r_sq3[j], r_gbc], w=[r_outt[j]])
            k.dma("sp", "out0", outd[(i - 1) * 128:i * 128, :], outt[j][:], r=[r_outt[j]])
        S.flush(nc)
    sH.close()
    gs.close()
    return finish()


def _tile_w(w, ncols):
    K_, N = w.shape
    return np.ascontiguousarray(w.reshape(K_ // 128, 128, N // ncols, ncols).transpose(2, 1, 0, 3))


def host_inputs(inputs):
    f32 = np.float32
    x = np.asarray(inputs["x"], f32)
    p = np.asarray(inputs["p"], f32)[0]
    w_in = np.asarray(inputs["w_in"], f32)[0]
    sp = np.cumsum([0, 1024, 256, 256, 256, 256, 256, 256, 24, 1024, 1024, 1024])
    nq, nkc, nvc, nks, nvs, nkw, nvw, ngate, dq, dk, dv = [w_in[:, sp[i]:sp[i + 1]] for i in range(11)]
    chunks = [dq[:, 0:512], dk[:, 0:512], dv[:, 0:512], dq[:, 512:], dk[:, 512:], dv[:, 512:],
              nq[:, 0:512], nq[:, 512:], np.concatenate([nkc, nks], 1), np.concatenate([nkw, nvc], 1),
              np.concatenate([nvs, nvw], 1)]
    w_inT = np.stack([_tile_w(c, 512)[0] for c in chunks])
    w_gT = _tile_w(np.concatenate([ngate, np.zeros((D, 8), f32)], 1), 32)[0]
    gains = np.zeros((8, D), f32)
    gains[0] = inputs["attn_norm"][0]
    gains[1] = inputs["ffn_norm"][0]
    gains[2] = inputs["ple_norm"][0]
    gains[3] = inputs["final_norm"]
    gains[4, :1024] = inputs["nsa_out_norm"][0]
    gains[5, :256] = inputs["diff_subln"][0]
    lamrow = np.concatenate([inputs["diff_lq1"][0], inputs["diff_lk1"][0], inputs["diff_lq2"][0], inputs["diff_lk2"][0]])[None, :].astype(f32)
    cw = np.asarray(inputs["conv_w"], f32)[0]
    cb = np.asarray(inputs["conv_b"], f32)[0]
    convw = np.ascontiguousarray(np.stack([cw[0], cw[1], cw[2], cb], axis=-1).reshape(2 * NFC, 128, 4).transpose(1, 0, 2))
    cw1T = np.stack([np.asarray(inputs[n][0], f32).reshape(32, 128, 256).transpose(1, 0, 2) for n in ("cmp_k_w1", "cmp_v_w1")])
    cw2T = np.stack([np.asarray(inputs[n][0], f32).reshape(2, 128, 128).transpose(1, 0, 2) for n in ("cmp_k_w2", "cmp_v_w2")])
    cposT = np.stack([np.asarray(inputs["cmp_k_pos"][0], f32).T, np.asarray(inputs["cmp_v_pos"][0], f32).T], axis=1)
    w_up = np.asarray(inputs["w_up"], f32)[0]
    w_upT = np.ascontiguousarray(
        np.concatenate([w_up[:, :D_FF].reshape(16, 128, NFC, 1, 128), w_up[:, D_FF:].reshape(16, 128, NFC, 1, 128)], axis=3)
        .transpose(2, 1, 0, 3, 4).reshape(NFC, 128, 16, 256))
    w_dn = np.asarray(inputs["w_down"], f32)[0]
    w_dnT = np.ascontiguousarray(w_dn.reshape(11, 4, 128, 4, 512).transpose(0, 3, 2, 1, 4))
    kk = np.arange(128)
    tri = (kk[:, None] <= kk[None, :]).astype(f32)
    tri2 = np.concatenate([tri, 1.0 - tri], axis=1).astype(ml_dtypes.bfloat16)
    ident = np.eye(128, dtype=f32).astype(ml_dtypes.bfloat16)
    cstart = np.arange(127) * 16
    sstart = np.arange(32) * 64
    overlap = (np.clip(np.minimum(cstart[:, None] + 32, sstart[None, :] + 64) - np.maximum(cstart[:, None], sstart[None, :]), 0, None) / 32.0).astype(f32)
    own_tok = 7 * 128 + np.arange(NOWN)
    cmpmask = (cstart[:, None] + 31 <= own_tok[None, :]).astype(f32).reshape(127, NT, 128).astype(ml_dtypes.bfloat16)
    expand = np.zeros((128, NSB, 128), f32)
    for s in range(NSB):
        for kq in range(128):
            expand[2 * s + kq // 64, s, kq] = 1.0
    expand = expand.astype(ml_dtypes.bfloat16)
    inv = (1.0 / (500000.0 ** (np.arange(0, 32, 2, dtype=f32) / f32(32.0)))).astype(f32)
    shared = dict(w_inT=w_inT, w_gT=np.ascontiguousarray(w_gT), gains=gains, lamrow=lamrow, convw=convw,
                  cw1T=np.ascontiguousarray(cw1T), cw2T=np.ascontiguousarray(cw2T), cposT=np.ascontiguousarray(cposT),
                  w_oT=_tile_w(np.asarray(inputs["w_o"], f32)[0], 512), w_upT=w_upT, w_dnT=w_dnT,
                  w_pgT=_tile_w(np.asarray(inputs["w_ple_gate"], f32)[0], 512),
                  w_ppT=_tile_w(np.asarray(inputs["w_ple_proj"], f32)[0], 512),
                  tri=tri2, ident=ident, overlap=overlap, cmpmask=cmpmask, expand=expand)
    maps = []
    for c in range(8):
        b, hh = c // 2, c % 2
        off = 0 if hh == 1 else -1024
        pos = np.arange(NSB * 128) + off
        real = pos >= 0
        if hh == 1:
            xe = x[b]
            pin = p[b, 1024:2048]
        else:
            xe = np.concatenate([np.zeros((1024, D), f32), x[b, :1024]], axis=0)
            pin = p[b, 0:1024]
        ang = (np.maximum(pos, 0).astype(f32))[:, None] * inv[None, :]
        cos = np.where(real[:, None], np.cos(ang), 1.0).astype(f32)
        sin = np.where(real[:, None], np.sin(ang), 0.0).astype(f32)
        cst = np.concatenate([cos, cos, sin, sin], axis=1)
        valid = np.ascontiguousarray(real.astype(f32).reshape(NSB, 128).T)
        validc = ((cstart + off) >= 0).astype(f32)[:, None]
        t_real = own_tok + off
        cur = np.floor_divide(t_real, 64)
        jr = np.arange(32) + off // 64
        forced = (jr[None, :] == 0) | (jr[None, :] == cur[:, None]) | (jr[None, :] == cur[:, None] - 1)
        future = (jr[None, :] > cur[:, None]) | (jr[None, :] < 0)
        sadd = np.where(forced, 1e4, np.where(future, -1e4, 0.0)).astype(f32)
        smul = np.where(forced | future, 0.0, 1.0).astype(f32)
        m = dict(shared)
        m.update(xe=np.ascontiguousarray(xe), pin=np.ascontiguousarray(pin), cst=cst, valid=valid, validc=validc,
                 smul=np.ascontiguousarray(smul.reshape(NT, 128, 32).transpose(1, 0, 2)),
                 sadd=np.ascontiguousarray(sadd.reshape(NT, 128, 32).transpose(1, 0, 2)))
        maps.append(m)
    return maps


def kernel(**inputs):
    maps = host_inputs(inputs)
    nc = build_program()
    res = run_bass_kernel_spmd(nc, maps, core_ids=list(range(8)))
    out = np.zeros((4, 2048, D), np.float32)
    for c in range(8):
        b, hh = c // 2, c % 2
        out[b, hh * 1024:(hh + 1) * 1024] = res.results[c]["out"]
    return out
```

```python
import os
from contextlib import ExitStack
import numpy as np
import ml_dtypes
import concourse.bass as bass
import concourse.mybir as mybir
from concourse.bass_utils import run_bass_kernel_spmd

F32 = mybir.dt.float32
BF16 = mybir.dt.bfloat16
AF = mybir.ActivationFunctionType
ALU = mybir.AluOpType
AX = mybir.AxisListType

D = 2048
NSB = 16
NT = 9
NOWN = NT * 128
HD = 128
EPS = 1e-6
SCALE = HD ** -0.5
D_FF = 5632
NFC = D_FF // 128
LAMBDA_INIT = 0.8 - 0.6
BIGNEG = -30000.0

ENGS = ["pe", "act", "dve", "pool", "sp"]
BLK = {"pe": "tensor", "act": "scalar", "dve": "vector", "pool": "gpsimd", "sp": "sync"}


class Res:
    __slots__ = ("name", "lw", "rd")

    def __init__(self, name):
        self.name = name
        self.lw = None
        self.rd = []


class Op:
    __slots__ = ("fn", "deps", "dma", "signal")

    def __init__(self, fn, deps, dma):
        self.fn = fn
        self.deps = deps
        self.dma = dma
        self.signal = False


class Sched:
    def __init__(self):
        self.ops = {e: [] for e in ENGS}
        self.seen = {e: {} for e in ENGS}
        self.dma_cnt = {}

    def add(self, eng, fn, r=(), w=(), dma=None):
        ops = self.ops[eng]
        idx = len(ops)
        if dma is not None:
            n = self.dma_cnt.get(dma, 0) + 1
            self.dma_cnt[dma] = n
            tok = ("d", dma, n)
        else:
            tok = ("e", eng, idx)
        deps = {}
        cand = []
        for x in r:
            if x.lw is not None:
                cand.append(x.lw)
        for x in w:
            if x.lw is not None:
                cand.append(x.lw)
            cand.extend(x.rd)
        for d in cand:
            if d[0] == "e" and d[1] == eng and eng == "pe":
                continue
            key = (d[0], d[1])
            if self.seen[eng].get(key, -1) >= d[2]:
                continue
            if deps.get(key, -1) < d[2]:
                deps[key] = d[2]
        need = []
        for key, v in deps.items():
            self.seen[eng][key] = v
            need.append((key[0], key[1], v))
            if key[0] == "e":
                self.ops[key[1]][v].signal = True
        ops.append(Op(fn, need, dma))
        for x in r:
            x.rd.append(tok)
        for x in w:
            x.lw = tok
            x.rd = []
        return tok

    def barrier(self):
        toks = []
        for e in ENGS:
            if self.ops[e]:
                n = len(self.ops[e]) - 1
                if self.ops[e][n].dma is None and self.ops[e][n].fn is not None:
                    toks.append(("e", e, n))
                else:
                    k = n
                    while k >= 0 and (self.ops[e][k].dma is not None or self.ops[e][k].fn is None):
                        k -= 1
                    if k >= 0:
                        toks.append(("e", e, k))
        for key, n in self.dma_cnt.items():
            toks.append(("d", key, n))
        for e in ENGS:
            need = []
            for d in toks:
                if d[0] == "e" and d[1] == e:
                    continue
                key = (d[0], d[1])
                if self.seen[e].get(key, -1) >= d[2]:
                    continue
                self.seen[e][key] = d[2]
                need.append(d)
                if d[0] == "e":
                    self.ops[d[1]][d[2]].signal = True
            self.ops[e].append(Op(None, need, None))

    def flush(self, nc):
        self.barrier()
        if not hasattr(self, "sem_e"):
            self.sem_e = {e: nc.alloc_semaphore(name="sem_" + e) for e in ENGS}
            self.sem_d = {}
            self.pos = {e: 0 for e in ENGS}
            self.cnt = {e: 0 for e in ENGS}
        for kname in self.dma_cnt:
            if kname not in self.sem_d:
                self.sem_d[kname] = nc.alloc_semaphore(name="semd_" + kname)
        sem_e, sem_d = self.sem_e, self.sem_d
        if not hasattr(self, "val"):
            self.val = {e: [] for e in ENGS}
        for e in ENGS:
            arr = self.val[e]
            c = self.cnt[e]
            for op in self.ops[e][len(arr):]:
                if op.signal and op.dma is None:
                    c += 1
                arr.append(c)
            self.cnt[e] = c
        val = self.val
        with nc.Block() as block:
            for e in ENGS:
                deco = getattr(block, BLK[e])
                lo = self.pos[e]
                seg = self.ops[e][lo:]
                self.pos[e] = len(self.ops[e])

                def body(engine, e=e, seg=seg):
                    for op in seg:
                        for d in op.deps:
                            if d[0] == "e":
                                engine.wait_ge(sem_e[d[1]], val[d[1]][d[2]])
                            else:
                                engine.wait_ge(sem_d[d[1]], 16 * d[2])
                        if op.fn is None:
                            continue
                        ins = op.fn(engine)
                        if op.dma is not None:
                            ins.then_inc(sem_d[op.dma], 16)
                        elif op.signal:
                            ins.then_inc(sem_e[e], 1)

                deco(body)


def bc(ap, n, axis=1):
    dims = [list(x) for x in ap.ap]
    dims.insert(axis, [0, n])
    return bass.AP(ap.tensor, ap.offset, dims)


def pbc(ap, n=128):
    dims = [list(x) for x in ap.ap]
    return bass.AP(ap.tensor, ap.offset, [[0, n]] + dims[1:])


class K:
    def __init__(self, nc):
        self.nc = nc
        self.S = Sched()
        self._n = 0

    def sb(self, stack, name, shape, dt, side=None):
        self._n += 1
        if side:
            return stack.enter_context(self.nc.sbuf_tensor(f"{name}_{self._n}", list(shape), dt, side=side))
        return stack.enter_context(self.nc.sbuf_tensor(f"{name}_{self._n}", list(shape), dt))

    def din(self, name, shape, dt=F32):
        return self.nc.dram_tensor(name, list(shape), dt, kind="ExternalInput").ap()

    def dma(self, q, key, out, in_, r=(), w=(), **kw):
        return self.S.add(q, lambda e: e.dma_start(out=out, in_=in_, **kw), r, w, dma=key)

    def mm(self, out, lhsT, rhs, start, stop, r=(), w=()):
        return self.S.add("pe", lambda e: e.matmul(out, lhsT, rhs, start=start, stop=stop), r, w)

    def tr(self, out, in_, ident, r=(), w=()):
        return self.S.add("pe", lambda e: e.transpose(out, in_, ident), r, w)

    def cp(self, eng, out, in_, r=(), w=()):
        if eng == "act":
            return self.S.add("act", lambda e: e.copy(out=out, in_=in_), r, w)
        return self.S.add(eng, lambda e: e.tensor_copy(out=out, in_=in_), r, w)

    def tt(self, out, in0, in1, op, r=(), w=(), eng="dve"):
        return self.S.add(eng, lambda e: e.tensor_tensor(out=out, in0=in0, in1=in1, op=op), r, w)

    def ts(self, out, in0, s1, s2, op0, op1=None, r=(), w=(), eng="dve"):
        if op1 is None:
            return self.S.add(eng, lambda e: e.tensor_scalar(out=out, in0=in0, scalar1=s1, scalar2=None, op0=op0), r, w)
        return self.S.add(eng, lambda e: e.tensor_scalar(out=out, in0=in0, scalar1=s1, scalar2=s2, op0=op0, op1=op1), r, w)

    def stt(self, out, in0, scalar, in1, op0, op1, r=(), w=()):
        return self.S.add("dve", lambda e: e.scalar_tensor_tensor(out=out, in0=in0, scalar=scalar, in1=in1, op0=op0, op1=op1), r, w)

    def af(self, out, in_, func, r=(), w=(), **kw):
        return self.S.add("act", lambda e: e.activation(out=out, in_=in_, func=func, **kw), r, w)

    def recip(self, out, in_, r=(), w=()):
        return self.S.add("dve", lambda e: e.reciprocal(out=out, in_=in_), r, w)


def build_program(dbg=None):
    nc = bass.Bass("TRN2", target_bir_lowering=False)
    k = K(nc)
    S = k.S
    gs = ExitStack()

    class _DD:
        pass
    dd = _DD()
    _specs = {
        'xe': ("xe", [NSB * 128, D]),
        'pin': ("pin", [1024, 256]),
        'cst': ("cst", [NSB * 128, 64]),
        'validd': ("valid", [128, NSB]),
        'validcd': ("validc", [127, 1]),
        'smuld': ("smul", [128, NT, 32]),
        'saddd': ("sadd", [128, NT, 32]),
        'cmpmd': ("cmpmask", [127, NT, 128], BF16),
        'ovld': ("overlap", [127, 32]),
        'expd': ("expand", [128, NSB, 128], BF16),
        'trid': ("tri", [128, 256], BF16),
        'identd': ("ident", [128, 128], BF16),
        'gains': ("gains", [8, D]),
        'lamd': ("lamrow", [1, 512]),
        'convd': ("convw", [128, 2 * NFC, 4]),
        'w_inT': ("w_inT", [11, 128, 16, 512]),
        'w_gT': ("w_gT", [128, 16, 32]),
        'cw1T': ("cw1T", [2, 128, 32, 256]),
        'cw2T': ("cw2T", [2, 128, 2, 128]),
        'cposT': ("cposT", [128, 2, 32]),
        'w_oT': ("w_oT", [4, 128, 16, 512]),
        'w_upT': ("w_upT", [NFC, 128, 16, 256]),
        'w_dnT': ("w_dnT", [11, 4, 128, 4, 512]),
        'w_pgT': ("w_pgT", [4, 128, 16, 512]),
        'w_ppT': ("w_ppT", [4, 128, 2, 512]),
    }

    def _lazy(self, name):
        if name.startswith("_") or name not in _specs:
            raise AttributeError(name)
        sp_ = _specs[name]
        ap = k.din(*sp_)
        setattr(self, name, ap)
        return ap
    _DD.__getattr__ = _lazy
    outd = nc.dram_tensor("out", [1024, D], F32, kind="ExternalOutput").ap()
    dbgd = None
    if dbg:
        dbgd = nc.dram_tensor("dbg", list(dbg[1]), F32, kind="ExternalOutput").ap()

    def finish():
        nc._used_inputs = [_specs[n][0] for n in dd.__dict__]
        return nc

    ps = [gs.enter_context(nc.psum_tensor(f"ps{i}", [128, 512], F32)) for i in range(8)]
    psr = [Res(f"ps{i}") for i in range(8)]

    def psT(b, n):
        return ps[b][:].bitcast(BF16)[:, 0:n * 128].rearrange("p (a b) -> p a b", a=n)

    _rpad = k.sb(gs, "rpad", [128, int(os.environ.get("RPAD", "4096"))], BF16, side="right")
    if os.environ.get("RTEST"):
        _rt = k.sb(gs, "rtest", [128, int(os.environ["RTEST"])], BF16, side="right")
        S.add("dve", lambda e: e.memset(_rt[:], 1.0), [], [Res("rt")])
    ident = k.sb(gs, "ident", [128, 128], BF16)
    tri2 = k.sb(gs, "tri2", [128, 256], BF16)
    cs = k.sb(gs, "cs", [128, NSB, 64], F32)
    valid = k.sb(gs, "valid", [128, NSB], F32)
    gbh = [None]

    def alloc_gbc(stack):
        gbh[0] = k.sb(stack, "gbc", [128, D], F32)
    lam = k.sb(gs, "lam", [128, 8], F32)
    sY = ExitStack()
    yTd = k.sb(sY, "yTd", [128, 8, NOWN], BF16)
    r_yTd = [Res(f"yTd{i}") for i in range(NT)]
    wbuf = [None, None]
    r_wbuf = [Res(f"wbuf{i}") for i in range(2)]

    def alloc_wbuf(stack):
        for i in range(2):
            wbuf[i] = k.sb(stack, f"wbuf{i}", [128, 16, 512], BF16)
    r_const = Res("const")
    r_gbc = Res("gbc")
    r_lam = Res("lam")

    k.dma("sp", "c0", ident[:], dd.identd, w=[r_const])
    k.dma("sp", "c0", tri2[:], dd.trid, w=[r_const])
    k.dma("sp", "c0", cs[:], dd.cst.rearrange("(s p) c -> p s c", p=128), w=[r_const])
    k.dma("sp", "c0", valid[:], dd.validd, w=[r_const])
    tri = tri2[:, 0:128]
    anti = tri2[:, 128:256]

    def load_gain(row, width=D):
        k.dma("sp", "gbc", gbh[0][:, 0:width], pbc(dd.gains[row:row + 1, 0:width]), w=[r_gbc])

    wcnt = [0]

    def load_w(src_ap, shape_cols):
        i = wcnt[0] % 2
        wcnt[0] += 1
        kcn, ncols = shape_cols
        k.dma("pool", f"wb{i}", wbuf[i][:, 0:kcn, 0:ncols], src_ap, w=[r_wbuf[i]], max_dma_last_dim=8192)
        return wbuf[i], r_wbuf[i]

    with ExitStack() as st:
        lrow = k.sb(st, "lrow", [128, 512], F32)
        ljunk = k.sb(st, "ljunk", [128, 128], F32)
        r_lrow = Res("lrow")
        k.dma("sp", "c1", lrow[:], pbc(dd.lamd), w=[r_lrow])
        for t in range(2):
            k.tt(ljunk[:], lrow[:, 256 * t:256 * t + 128], lrow[:, 256 * t + 128:256 * t + 256], ALU.mult, r=[r_lrow], w=[r_lam])
            S.add("dve", lambda e, t=t: e.reduce_sum(out=lam[:, 1 + t:2 + t], in_=ljunk[:], axis=AX.X), [r_lam], [r_lam])
        k.af(lam[:, 3:5], lam[:, 1:3], AF.Exp, r=[r_lam], w=[r_lam])
        k.tt(lam[:, 5:6], lam[:, 4:5], lam[:, 3:4], ALU.subtract, r=[r_lam], w=[r_lam])
        k.ts(lam[:, 0:1], lam[:, 5:6], -LAMBDA_INIT, None, ALU.add, r=[r_lam], w=[r_lam])
        S.flush(nc)
    neglam = lam[:, 0:1]

    def rms_rstd(src_ap, ncols, junk, ss, r_src, r_junk, r_ss, mult=1.0):
        k.af(junk, src_ap, AF.Square, r=[r_src], w=[r_junk, r_ss], accum_out=ss[:, 0:1])
        k.af(ss[:, 2:3], ss[:, 0:1], AF.Sqrt, r=[r_ss], w=[r_ss], scale=1.0 / (ncols * mult * mult), bias=EPS / (mult * mult))
        k.recip(ss[:, 1:2], ss[:, 2:3], r=[r_ss], w=[r_ss])

    def norm_transpose(stack_bufs, src_ap, r_src, dst_of_q4, r_dst, nkc=16, pbase=0):
        xs, r_xs, sq, r_sq = stack_bufs
        rms_rstd(src_ap, nkc * 128, xs[:, 0:nkc * 128], sq, r_src, r_xs, r_sq)
        k.stt(xs[:, 0:nkc * 128], src_ap, sq[:, 1:2], gbh[0][:, 0:nkc * 128], ALU.mult, ALU.mult, r=[r_src, r_sq, r_gbc], w=[r_xs])
        for q4 in range(nkc // 4):
            pb = pbase + q4 % 2
            pT = psT(pb, 4)
            for a in range(4):
                kc = q4 * 4 + a
                k.tr(pT[:, a, :], xs[:, kc * 128:(kc + 1) * 128], ident[:], r=[r_xs, r_const], w=[psr[pb]])
            k.cp("act" if q4 % 2 == 0 else "dve", dst_of_q4(q4), pT, r=[psr[pb]], w=r_dst)

    p12 = ExitStack()
    xnT = k.sb(p12, "xnT", [128, 16, NSB * 128], BF16)
    r_xnT = [Res(f"xnT{s}") for s in range(NSB)]
    Tst = [k.sb(p12, f"Tst{i}", [128, 4, 128], BF16) for i in range(2)]
    r_T = [Res("T0"), Res("T1")]
    rtmp = [k.sb(p12, f"rtmp{i}", [128, 2, 4, 32], F32) for i in range(2)]
    alloc_wbuf(p12)

    with ExitStack() as st:
        xt = [k.sb(st, f"xt{i}", [128, D], F32) for i in range(2)]
        xs = [k.sb(st, f"xs{i}", [128, D], BF16) for i in range(2)]
        sq = [k.sb(st, f"sq{i}", [128, 4], F32) for i in range(2)]
        r_xt = [Res("xt0"), Res("xt1")]
        r_xs = [Res("xs0"), Res("xs1")]
        r_sq = [Res("sq0"), Res("sq1")]
        alloc_gbc(st)
        load_gain(0)
        for s in range(NSB):
            j = s % 2
            k.dma("sp", f"xt{j}", xt[j][:], dd.xe[s * 128:(s + 1) * 128, :], w=[r_xt[j]])
            norm_transpose((xs[j], r_xs[j], sq[j], r_sq[j]), xt[j][:], r_xt[j],
                           lambda q4, s=s: xnT[:, q4 * 4:(q4 + 1) * 4, s * 128:(s + 1) * 128], [r_xnT[s]])
        S.flush(nc)

    pcnt = [0]
    tcnt = [0]
    r_ser = [Res("ser0"), Res("ser1")]

    def project(wb, r_wb, ncols, blocks, evac):
        for sbk in blocks:
            pb = 2 + (pcnt[0] % 6)
            pcnt[0] += 1
            for kc in range(16):
                k.mm(ps[pb][:, 0:ncols], xnT[:, kc, sbk * 128:(sbk + 1) * 128], wb[:, kc, 0:ncols],
                     start=(kc == 0), stop=(kc == 15), r=[r_xnT[sbk], r_wb], w=[psr[pb]])
            evac(sbk, ps[pb], psr[pb])

    def rope_transpose(psb, r_psb, sbk, nheads, nrope, dsts):
        j = tcnt[0] % 2
        tcnt[0] += 1
        T = Tst[j]
        src = psb[:, 0:nheads * 128].rearrange("p (h d) -> p h d", h=nheads)
        if nrope < nheads:
            k.cp("act", T[:, nrope:nheads, :], src[:, nrope:nheads, :], r=[r_psb], w=[r_T[j]])
        if nrope > 0:
            cc = bc(cs[:, sbk, 0:32], nrope)
            sn = bc(cs[:, sbk, 32:64], nrope)
            tc_ = rtmp[j][:, 0, 0:nrope, :]
            ts_ = rtmp[j][:, 1, 0:nrope, :]
            k.cp("act", T[:, 0:nrope, 32:128], src[:, 0:nrope, 32:128], r=[r_psb], w=[r_T[j]])
            k.tt(tc_, src[:, 0:nrope, 0:32], cc, ALU.mult, r=[r_psb, r_const], w=[r_T[j]])
            k.tt(ts_, src[:, 0:nrope, 0:32], sn, ALU.mult, r=[r_psb, r_const], w=[r_T[j]])
            k.tt(T[:, 0:nrope, 0:16], tc_[:, :, 0:16], ts_[:, :, 16:32], ALU.subtract, r=[r_T[j]], w=[r_T[j]])
            k.tt(T[:, 0:nrope, 16:32], tc_[:, :, 16:32], ts_[:, :, 0:16], ALU.add, r=[r_T[j]], w=[r_T[j]])
        pb = tcnt[0] % 2
        pT = psT(pb, 4)
        for h in range(nheads):
            k.tr(pT[:, h, :], T[:, h, :], ident[:], r=[r_T[j], r_const], w=[psr[pb]])
        for n, (h0, hh1, dst, r_dst) in enumerate(dsts):
            k.cp("act" if n == 0 else "dve", dst, pT[:, h0:hh1, :], r=[psr[pb]], w=list(r_dst) + [r_ser[pb]])

    own_blocks = list(range(7, 16))
    all_blocks = list(range(NSB))

    with ExitStack() as sA:
        dKT = k.sb(sA, "dKT", [128, 4, NSB * 128], BF16)
        dVa = k.sb(sA, "dVa", [128, NSB, 2, 257], BF16)
        dQT = k.sb(sA, "dQT", [128, 4, NOWN], BF16)
        r_dKT = [Res(f"dKT{s}") for s in range(NSB)]
        r_dVa = [Res(f"dVa{s}") for s in range(NSB)]
        r_dQT = [Res(f"dQT{i}") for i in range(NT)]
        Eb = [k.sb(sA, f"E{i}", [128, 384], BF16) for i in range(3)]
        r_E = [Res(f"E{i}") for i in range(3)]
        o1 = [k.sb(sA, f"o1_{i}", [128, 256], F32) for i in range(3)]
        r_o1 = [Res(f"o1_{i}") for i in range(3)]
        od = [k.sb(sA, f"od{i}", [128, 256], F32) for i in range(2)]
        r_od = [Res("od0"), Res("od1")]
        ydn = [k.sb(sA, f"ydn{i}", [128, 256], BF16) for i in range(2)]
        r_ydn = [Res("ydn0"), Res("ydn1")]
        rr = [k.sb(sA, f"rr{i}", [128, 8], F32) for i in range(2)]
        r_rr = [Res("rr0"), Res("rr1")]
        ojunk = k.sb(sA, "ojunk", [128, 256], BF16)
        r_ojunk = Res("ojunk")
        alloc_gbc(sA)
        load_gain(5, 256)
        ecnt = 0
        fcnt = 0
        scnt = 0
        for hp in range(2):
            wb, r_wb = load_w(dd.w_inT[3 * hp + 0], (16, 512))

            def ev(sbk, psb, r_psb):
                i = sbk - 7
                rope_transpose(psb, r_psb, sbk, 4, 4, [(0, 4, dQT[:, :, i * 128:(i + 1) * 128], [r_dQT[i]])])
            project(wb, r_wb, 512, own_blocks, ev)
            wb, r_wb = load_w(dd.w_inT[3 * hp + 1], (16, 512))

            def ev(sbk, psb, r_psb):
                rope_transpose(psb, r_psb, sbk, 4, 4, [(0, 4, dKT[:, :, sbk * 128:(sbk + 1) * 128], [r_dKT[sbk]])])
            project(wb, r_wb, 512, all_blocks, ev)
            wb, r_wb = load_w(dd.w_inT[3 * hp + 2], (16, 512))

            def ev(sbk, psb, r_psb):
                src = psb[:, 0:512].rearrange("p (h d) -> p h d", h=2)
                k.cp("act", dVa[:, sbk, :, 0:256], src, r=[r_psb], w=[r_dVa[sbk]])
                k.cp("dve", dVa[:, sbk, :, 256:257], bc(valid[:, sbk:sbk + 1], 2), r=[r_const], w=[r_dVa[sbk]])
            project(wb, r_wb, 512, all_blocks, ev)

            for hl in range(2):
                h = 2 * hp + hl
                for g in range(3):
                    lo = 3 * g
                    for m in range(2):
                        hm = 2 * hl + m
                        for s in range(7 + lo + 3):
                            i0 = max(lo, s - 7)
                            nq = (lo + 3 - i0) * 128
                            sp_ = scnt % 2
                            scnt += 1
                            k.mm(ps[sp_][:, 0:nq], dKT[:, hm, s * 128:(s + 1) * 128], dQT[:, hm, i0 * 128:(lo + 3) * 128],
                                 start=True, stop=True, r=[r_dKT[s]] + [r_dQT[i] for i in range(i0, lo + 3)], w=[psr[sp_]])
                            ej = ecnt % 3
                            ecnt += 1
                            E = Eb[ej]
                            k.af(E[:, 0:nq], ps[sp_][:, 0:nq], AF.Exp, r=[psr[sp_]], w=[r_E[ej]], scale=SCALE)
                            if s - 7 >= lo:
                                k.tt(E[:, 0:128], E[:, 0:128], tri, ALU.mult, r=[r_E[ej], r_const], w=[r_E[ej]])
                            for i in range(i0, lo + 3):
                                a = i - lo
                                acc = ps[2 + a]
                                r_acc = psr[2 + a]
                                k.mm(acc[:, 0:257], E[:, (i - i0) * 128:(i - i0 + 1) * 128], dVa[:, s, hl, :],
                                     start=(s == 0), stop=(s == 7 + i), r=[r_E[ej], r_dVa[s]], w=[r_acc])
                                if s != 7 + i:
                                    continue
                                fj = fcnt % 2
                                if m == 0:
                                    k.ts(rr[fj][:, 5:6], acc[:, 256:257], 1e-30, None, ALU.max, r=[r_acc], w=[r_rr[fj]])
                                    k.recip(rr[fj][:, 0:1], rr[fj][:, 5:6], r=[r_rr[fj]], w=[r_rr[fj]])
                                    k.ts(o1[a][:], acc[:, 0:256], rr[fj][:, 0:1], None, ALU.mult, r=[r_acc, r_rr[fj]], w=[r_o1[a]])
                                    continue
                                fcnt += 1
                                k.ts(rr[fj][:, 5:6], acc[:, 256:257], 1e-30, None, ALU.max, r=[r_acc], w=[r_rr[fj]])
                                k.recip(rr[fj][:, 0:1], rr[fj][:, 5:6], r=[r_rr[fj]], w=[r_rr[fj]])
                                k.tt(rr[fj][:, 1:2], rr[fj][:, 0:1], neglam, ALU.mult, r=[r_rr[fj], r_lam], w=[r_rr[fj]])
                                k.stt(od[fj][:], acc[:, 0:256], rr[fj][:, 1:2], o1[a][:], ALU.mult, ALU.add,
                                      r=[r_acc, r_rr[fj], r_o1[a]], w=[r_od[fj]])
                                if dbg and dbg[0] == "ydiff":
                                    k.dma("sp", "dbg", dbgd[i * 128:(i + 1) * 128, h * 256:(h + 1) * 256], od[fj][:], r=[r_od[fj]])
                                k.af(ojunk[:], od[fj][:], AF.Square, r=[r_od[fj]], w=[r_ojunk, r_rr[fj]], accum_out=rr[fj][:, 2:3])
                                mlt = 1.0 - LAMBDA_INIT
                                k.af(rr[fj][:, 3:4], rr[fj][:, 2:3], AF.Sqrt, r=[r_rr[fj]], w=[r_rr[fj]],
                                     scale=1.0 / (256 * mlt * mlt), bias=EPS / (mlt * mlt))
                                k.recip(rr[fj][:, 4:5], rr[fj][:, 3:4], r=[r_rr[fj]], w=[r_rr[fj]])
                                k.stt(ydn[fj][:], od[fj][:], rr[fj][:, 4:5], gbh[0][:, 0:256], ALU.mult, ALU.mult,
                                      r=[r_od[fj], r_rr[fj], r_gbc], w=[r_ydn[fj]])
                                pT = psT(5 + fj, 2)
                                for c2 in range(2):
                                    k.tr(pT[:, c2, :], ydn[fj][:, c2 * 128:(c2 + 1) * 128], ident[:], r=[r_ydn[fj], r_const], w=[psr[5 + fj]])
                                k.cp("act", yTd[:, 2 * h:2 * h + 2, i * 128:(i + 1) * 128], pT, r=[psr[5 + fj]], w=[r_yTd[i]])
        S.flush(nc)

    if dbg and dbg[0] == "ydiff":
        return finish()


    sBp = ExitStack()
    KnT = k.sb(sBp, "KnT", [128, 4, 2, NSB * 128], BF16, side="right")
    Vau = k.sb(sBp, "Vau", [128, NSB, 2, 2, 129], BF16, side="right")
    vcT = KnT[:, 3]
    QnT = k.sb(sBp, "QnT", [128, NT, 2, 512], BF16, side="right")
    gate = k.sb(sBp, "gate", [128, NT, 24], F32, side="right")
    r_KnT = [Res(f"KnT{s}") for s in range(NSB)]
    r_Vau = [Res(f"Vau{s}") for s in range(NSB)]
    r_vcT = [Res(f"vcT{s}") for s in range(NSB)]
    r_QnT = [Res(f"QnT{i}") for i in range(NT)]
    r_gate = [Res(f"gate{i}") for i in range(NT)]

    for kv in range(2):
        wb, r_wb = load_w(dd.w_inT[6 + kv], (16, 512))

        def ev(sbk, psb, r_psb, kv=kv):
            i = sbk - 7
            rope_transpose(psb, r_psb, sbk, 4, 4,
                           [(0, 4, QnT[:, i, kv, :].rearrange("p (g q) -> p g q", g=4), [r_QnT[i]])])
        project(wb, r_wb, 512, own_blocks, ev)
    if dbg and dbg[0] == "b1":
        S.flush(nc)
        return finish()
    wb, r_wb = load_w(dd.w_inT[8], (16, 512))

    def ev(sbk, psb, r_psb):
        sl = slice(sbk * 128, (sbk + 1) * 128)
        rope_transpose(psb, r_psb, sbk, 4, 4, [(0, 2, KnT[:, 0, :, sl], [r_KnT[sbk]]), (2, 4, KnT[:, 1, :, sl], [r_KnT[sbk]])])
    project(wb, r_wb, 512, all_blocks, ev)
    if dbg and dbg[0] == "b2":
        S.flush(nc)
        return finish()
    wb, r_wb = load_w(dd.w_inT[9], (16, 512))

    def ev(sbk, psb, r_psb):
        sl = slice(sbk * 128, (sbk + 1) * 128)
        rope_transpose(psb, r_psb, sbk, 4, 2, [(0, 2, KnT[:, 2, :, sl], [r_KnT[sbk]]), (2, 4, vcT[:, :, sl], [r_vcT[sbk]])])
    project(wb, r_wb, 512, all_blocks, ev)
    if dbg and dbg[0] == "b3":
        S.flush(nc)
        return finish()
    wb, r_wb = load_w(dd.w_inT[10], (16, 512))

    def ev(sbk, psb, r_psb):
        src = psb[:, 0:512].rearrange("p (b h d) -> p b h d", b=2, h=2)
        for br in range(2):
            k.cp("act" if br == 0 else "dve", Vau[:, sbk, br, :, 0:128], src[:, br], r=[r_psb], w=[r_Vau[sbk]])
            k.cp("dve", Vau[:, sbk, br, :, 128:129], bc(valid[:, sbk:sbk + 1], 2), r=[r_const], w=[r_Vau[sbk]])
    project(wb, r_wb, 512, all_blocks, ev)
    if dbg and dbg[0] == "b4":
        S.flush(nc)
        return finish()
    wb, r_wb = load_w(dd.w_gT, (16, 32))

    def ev(sbk, psb, r_psb):
        i = sbk - 7
        k.af(gate[:, i, :], psb[:, 0:24], AF.Sigmoid, r=[r_psb], w=[r_gate[i]])
    project(wb, r_wb, 32, own_blocks, ev)
    S.flush(nc)
    if dbg and dbg[0] == "bproj":
        return finish()
    p12.close()

    sB2 = ExitStack()
    yTn = k.sb(sB2, "yTn", [128, 8, NOWN], BF16)
    r_yTn = [Res(f"yTn{i}") for i in range(NT)]
    with ExitStack() as st:
        cmpm = k.sb(st, "cmpm", [128, NT, 128], BF16)
        ovl = k.sb(st, "ovl", [128, 32], F32)
        expn = k.sb(st, "expn", [128, NSB, 128], BF16)
        smul = k.sb(st, "smul", [128, NT, 32], F32)
        sadd = k.sb(st, "sadd", [128, NT, 32], F32)
        vldc = k.sb(st, "vldc", [128, 1], F32)
        posT = k.sb(st, "posT", [128, 2, 32], F32)
        KcT = k.sb(st, "KcT", [128, 2, 128], BF16)
        Rc = k.sb(st, "Rc", [128, 2, 161], BF16)
        r_c2 = Res("c2")
        r_KcT = Res("KcT")
        r_Rc = Res("Rc")
        k.dma("sp", "c2", cmpm[0:127], dd.cmpmd, w=[r_c2])
        k.dma("sp", "c2", ovl[0:127], dd.ovld, w=[r_c2])
        k.dma("sp", "c2", expn[:], dd.expd, w=[r_c2])
        k.dma("sp", "c2", smul[:], dd.smuld, w=[r_c2])
        k.dma("sp", "c2", sadd[:], dd.saddd, w=[r_c2])
        k.dma("sp", "c2", vldc[0:127], dd.validcd, w=[r_c2])
        k.dma("sp", "c2", posT[:], dd.cposT, w=[r_c2])

        with ExitStack() as st2:
            w1 = k.sb(st2, "w1", [128, 32, 256], BF16)
            w2 = k.sb(st2, "w2", [128, 2, 128], BF16)
            A2 = [k.sb(st2, f"A2{ab}", [128, 16, 128], BF16) for ab in range(2)]
            hs = k.sb(st2, "hs", [128, 128], F32)
            uu = k.sb(st2, "uu", [128, 128], F32)
            hT = k.sb(st2, "hT", [128, 2, 128], BF16)
            r_w1, r_w2, r_hs, r_uu, r_hT = Res("w1"), Res("w2"), Res("hs"), Res("uu"), Res("hT")
            r_A = [Res("A20"), Res("A21")]
            for t in range(2):
                k.dma("pool", "w1", w1[:], dd.cw1T[t], w=[r_w1], max_dma_last_dim=8192)
                k.dma("pool", "w2", w2[:], dd.cw2T[t], w=[r_w2])
                for kv in range(2):
                    if t == 0:
                        src = KnT[:, 0, kv, :].rearrange("p (g l) -> p g l", l=16)
                        r_src = r_KnT
                    else:
                        src = vcT[:, kv, :].rearrange("p (g l) -> p g l", l=16)
                        r_src = r_vcT
                    for ab in range(2):
                        k.tt(A2[ab][:].rearrange("p l g -> p g l"), src, bc(posT[:, t, 16 * ab:16 * ab + 16], 128, axis=1), ALU.add,
                             r=list(r_src) + [r_c2], w=[r_A[ab]])
                    for nch in range(2):
                        pb = 6 + nch
                        for l in range(32):
                            rhs = A2[0][:, l, 0:127] if l < 16 else A2[1][:, l - 16, 1:128]
                            k.mm(ps[pb][:, 0:127], w1[:, l, nch * 128:(nch + 1) * 128], rhs, start=(l == 0), stop=(l == 31),
                                 r=[r_w1, r_A[0], r_A[1]], w=[psr[pb]])
                        k.cp("act", hs[:, 0:127], ps[pb][:, 0:127], r=[psr[pb]], w=[r_hs])
                        k.tt(uu[:, 0:127], hs[:, 0:127], hs[:, 0:127], ALU.mult, r=[r_hs], w=[r_uu])
                        k.ts(uu[:, 0:127], uu[:, 0:127], 0.044715, 1.0, ALU.mult, ALU.add, r=[r_uu], w=[r_uu])
                        k.tt(uu[:, 0:127], uu[:, 0:127], hs[:, 0:127], ALU.mult, r=[r_uu, r_hs], w=[r_uu])
                        k.af(uu[:, 0:127], uu[:, 0:127], AF.Sigmoid, r=[r_uu], w=[r_uu], scale=1.5957691216057308)
                        k.tt(hT[:, nch, 0:127], uu[:, 0:127], hs[:, 0:127], ALU.mult, r=[r_uu, r_hs], w=[r_hT])
                    pb = 5
                    if t == 0:
                        for nch in range(2):
                            k.mm(ps[pb][:, 0:127], w2[:, nch, :], hT[:, nch, 0:127], start=(nch == 0), stop=(nch == 1),
                                 r=[r_w2, r_hT], w=[psr[pb]])
                        k.cp("act", KcT[:, kv, 0:127], ps[pb][:, 0:127], r=[psr[pb]], w=[r_KcT])
                    else:
                        for nch in range(2):
                            k.mm(ps[pb][0:127, 0:128], hT[:, nch, 0:127], w2[:, nch, :], start=(nch == 0), stop=(nch == 1),
                                 r=[r_w2, r_hT], w=[psr[pb]])
                        k.ts(Rc[0:127, kv, 0:128], ps[pb][0:127, 0:128], vldc[0:127, 0:1], None, ALU.mult, r=[psr[pb], r_c2], w=[r_Rc])
                        k.cp("dve", Rc[0:127, kv, 128:129], vldc[0:127, 0:1], r=[r_c2], w=[r_Rc])
                        k.ts(Rc[0:127, kv, 129:161], ovl[0:127, :], vldc[0:127, 0:1], None, ALU.mult, r=[r_c2], w=[r_Rc])
            S.flush(nc)
            if dbg and dbg[0] == "cmp":
                return finish()

        with ExitStack() as st2:
            Eb = [k.sb(st2, f"En{i}", [128, 512], BF16) for i in range(3)]
            r_E = [Res(f"En{i}") for i in range(3)]
            onsa = [k.sb(st2, f"onsa{i}", [128, 8, 128], F32) for i in range(2)]
            r_onsa = [Res("onsa0"), Res("onsa1")]
            scl = k.sb(st2, "scl", [128, 16], F32)
            psl = k.sb(st2, "psl", [128, 32], F32)
            scr = k.sb(st2, "scr", [128, 32], F32)
            sc2 = k.sb(st2, "sc2", [128, 32], F32)
            m8 = k.sb(st2, "m8", [128, 16], F32)
            selb = k.sb(st2, "selb", [128, 128], BF16)
            selT = k.sb(st2, "selT", [128, 4, 128], BF16)
            xsn = k.sb(st2, "xsn", [128, 1024], BF16)
            sqn = k.sb(st2, "sqn", [128, 4], F32)
            r_scl, r_sel, r_selT, r_xsn, r_sqn = Res("scl"), Res("sel"), Res("selT"), Res("xsn"), Res("sqn")
            alloc_gbc(st2)
            load_gain(4, 1024)
            S.add("dve", lambda e: e.memset(selb[:], 0.0), [], [r_sel])
            S.add("dve", lambda e: e.memset(selT[:], 0.0), [], [r_selT])
            cnt = {"s": 0, "e": 0}

            def next_s():
                cnt["s"] += 1
                return cnt["s"] % 2

            def next_e():
                cnt["e"] += 1
                return cnt["e"] % 3

            def fin_scalars(lcols, i, kv, gbr):
                for g, (ap_l, r_l) in enumerate(lcols):
                    k.ts(scl[:, g:g + 1], ap_l, 1e-30, None, ALU.max, r=[r_l], w=[r_scl])
                k.recip(scl[:, 4:8], scl[:, 0:4], r=[r_scl], w=[r_scl])
                k.tt(scl[:, 8:12], scl[:, 4:8], gate[:, i, gbr * 8 + kv * 4:gbr * 8 + kv * 4 + 4], ALU.mult, r=[r_scl, r_gate[i]], w=[r_scl])

            def branch(i, kv, oj, slots, kbr, vbr, gbr, first_anti, use_bias):
                Q = QnT[:, i, kv, :]
                for n, s in enumerate(slots):
                    sp_ = next_s()
                    k.mm(ps[sp_][:, :], KnT[:, kbr, kv, s * 128:(s + 1) * 128], Q, start=True, stop=not use_bias,
                         r=[r_KnT[s], r_QnT[i]], w=[psr[sp_]])
                    if use_bias:
                        k.mm(ps[sp_][:, :], expn[:, s, :], selT[:].rearrange("p g q -> p (g q)"), start=False, stop=True,
                             r=[r_c2, r_selT], w=[psr[sp_]])
                    ej = next_e()
                    E = Eb[ej]
                    k.af(E[:, :], ps[sp_][:, :], AF.Exp, r=[psr[sp_]], w=[r_E[ej]], scale=SCALE)
                    msk = tri if s == 7 + i else (anti if (first_anti and n == 0) else None)
                    if msk is not None:
                        E4 = E[:, :].rearrange("p (g q) -> p g q", g=4)
                        k.tt(E4, E4, bc(msk, 4), ALU.mult, r=[r_E[ej], r_const], w=[r_E[ej]])
                    for g in range(4):
                        k.mm(ps[2 + g][:, 0:129], E[:, g * 128:(g + 1) * 128], Vau[:, s, vbr, kv, :], start=(n == 0),
                             stop=(n == len(slots) - 1), r=[r_E[ej], r_Vau[s]], w=[psr[2 + g]])
                fin_scalars([(ps[2 + g][:, 128:129], psr[2 + g]) for g in range(4)], i, kv, gbr)
                for g in range(4):
                    o_ = onsa[oj][:, kv * 4 + g, :]
                    k.stt(o_, ps[2 + g][:, 0:128], scl[:, 8 + g:9 + g], o_, ALU.mult, ALU.add, r=[psr[2 + g], r_scl], w=[r_onsa[oj]])

            for i in range(NT):
                oj = i % 2
                for kv in range(2):
                    Q = QnT[:, i, kv, :]
                    sp_ = next_s()
                    k.mm(ps[sp_][0:127, :], KcT[:, kv, 0:127], Q, start=True, stop=True, r=[r_KcT, r_QnT[i]], w=[psr[sp_]])
                    ej = next_e()
                    E = Eb[ej]
                    k.af(E[0:127, :], ps[sp_][0:127, :], AF.Exp, r=[psr[sp_]], w=[r_E[ej]], scale=SCALE)
                    E4 = E[0:127, :].rearrange("p (g q) -> p g q", g=4)
                    k.tt(E4, E4, bc(cmpm[0:127, i, :], 4), ALU.mult, r=[r_E[ej], r_c2], w=[r_E[ej]])
                    C = []
                    for g in range(4):
                        cb = 6 + g // 2
                        cap = ps[cb][:, (g % 2) * 161:(g % 2) * 161 + 161]
                        k.mm(cap, E[0:127, g * 128:(g + 1) * 128], Rc[0:127, kv, :], start=True, stop=True, r=[r_E[ej], r_Rc], w=[psr[cb]])
                        C.append((cap, psr[cb]))
                    fin_scalars([(C[g][0][:, 128:129], C[g][1]) for g in range(4)], i, kv, 0)
                    for g in range(4):
                        k.ts(onsa[oj][:, kv * 4 + g, :], C[g][0][:, 0:128], scl[:, 8 + g:9 + g], None, ALU.mult,
                             r=[C[g][1], r_scl], w=[r_onsa[oj]])
                    k.ts(psl[:], C[0][0][:, 129:161], scl[:, 4:5], None, ALU.mult, r=[C[0][1], r_scl], w=[r_sel])
                    for g in range(1, 4):
                        k.stt(psl[:], C[g][0][:, 129:161], scl[:, 4 + g:5 + g], psl[:], ALU.mult, ALU.add, r=[C[g][1], r_scl, r_sel], w=[r_sel])
                    k.tt(scr[:], psl[:], smul[:, i, :], ALU.mult, r=[r_sel, r_c2], w=[r_sel])
                    k.tt(scr[:], scr[:], sadd[:, i, :], ALU.add, r=[r_sel, r_c2], w=[r_sel])
                    S.add("dve", lambda e: e.max(out=m8[:, 0:8], in_=scr[:]), [r_sel], [r_sel])
                    S.add("dve", lambda e: e.match_replace(out=sc2[:], in_to_replace=m8[:, 0:8], in_values=scr[:], imm_value=-1e9),
                          [r_sel], [r_sel])
                    S.add("dve", lambda e: e.max(out=m8[:, 8:16], in_=sc2[:]), [r_sel], [r_sel])
                    k.ts(selb[:, 0:32], scr[:], m8[:, 15:16], BIGNEG, ALU.is_lt, ALU.mult, r=[r_sel], w=[r_sel])
                    tb = next_s()
                    pTs = ps[tb][:].bitcast(BF16)[:, 0:128]
                    k.tr(pTs, selb[:], ident[:], r=[r_sel, r_const], w=[psr[tb]])
                    k.cp("act", selT[0:32], bc(pTs[0:32, :], 4), r=[psr[tb]], w=[r_selT])
                    branch(i, kv, oj, list(range(3 + i, 8 + i)), 2, 1, 2, True, False)
                    branch(i, kv, oj, list(range(0, 8 + i)), 1, 0, 1, False, True)
                norm_transpose((xsn, r_xsn, sqn, r_sqn), onsa[oj][:].rearrange("p h d -> p (h d)"), r_onsa[oj],
                               lambda q4, i=i: yTn[:, q4 * 4:(q4 + 1) * 4, i * 128:(i + 1) * 128], [r_yTn[i]], nkc=8, pbase=6)
                if dbg and dbg[0] == "nsa1" and i == 0:
                    break
            S.flush(nc)
            if dbg and dbg[0] in ("nsa", "nsa1"):
                return finish()
    sBp.close()

    sH = ExitStack()
    h1 = k.sb(sH, "h1", [128, NT, D], F32, side="right")
    r_h1 = [Res(f"h1_{i}") for i in range(NT)]
    r_h1all = Res("h1all")
    with ExitStack() as st:
        alloc_wbuf(st)
        for i in range(NT):
            k.dma("sp", "h1l", h1[:, i, :], dd.xe[(7 + i) * 128:(8 + i) * 128, :], w=[r_h1[i], r_h1all])
        for c4 in range(4):
            wb, r_wb = load_w(dd.w_oT[c4], (16, 512))
            for i in range(NT):
                pb = 2 + (pcnt[0] % 3)
                pcnt[0] += 1
                for kc in range(16):
                    lhsT = yTn[:, kc, i * 128:(i + 1) * 128] if kc < 8 else yTd[:, kc - 8, i * 128:(i + 1) * 128]
                    k.mm(ps[pb][:, :], lhsT, wb[:, kc, :], start=(kc == 0), stop=(kc == 15), r=[r_yTn[i], r_yTd[i], r_wb], w=[psr[pb]])
                hv = h1[:, i, c4 * 512:(c4 + 1) * 512]
                k.tt(hv, ps[pb][:, :], hv, ALU.add, r=[psr[pb], r_h1all, r_h1[i]], w=[r_h1[i]])
        S.flush(nc)
    sB2.close()
    sY.close()
    if dbg and dbg[0] in ("hh", "h1"):
        k.dma("sp", "dbg", dbgd[0].rearrange("(i p) c -> p i c", p=128), h1[:], r=r_h1)
        S.flush(nc)
        if dbg[0] == "h1":
            return finish()

    with ExitStack() as sF:
        xn2T = k.sb(sF, "xn2T", [128, 16, NOWN], BF16)
        r_xn2T = [Res(f"xn2T{i}") for i in range(NT)]
        _xs2 = k.sb(sF, "xs2", [128, D], BF16)
        xs2 = [_xs2, _xs2]
        sq2 = [k.sb(sF, f"sq2{j}", [128, 4], F32) for j in range(2)]
        _rxs2 = Res("xs2")
        r_xs2 = [_rxs2, _rxs2]
        r_sq2 = [Res("sq20"), Res("sq21")]
        cvw = k.sb(sF, "cvw", [128, 2 * NFC, 4], F32)
        r_c3 = Res("c3")
        k.dma("sp", "c3", cvw[:], dd.convd, w=[r_c3])
        alloc_gbc(sF)
        load_gain(1)
        for i in range(NT):
            j = i % 2
            norm_transpose((xs2[j], r_xs2[j], sq2[j], r_sq2[j]), h1[:, i, :], r_h1[i],
                           lambda q4, i=i: xn2T[:, q4 * 4:(q4 + 1) * 4, i * 128:(i + 1) * 128], [r_xn2T[i]], pbase=0)
        raw = [[k.sb(sF, f"raw{j}{t}", [128, 2 + NOWN], F32) for t in range(2)] for j in range(2)]
        r_raw = [[Res(f"raw{j}{t}") for t in range(2)] for j in range(2)]
        yv = [k.sb(sF, f"yv{t}", [128, NOWN], F32) for t in range(2)]
        r_yv = [Res("yv0"), Res("yv1")]
        aT = [k.sb(sF, f"aT{j}", [128, 4, 1024], BF16) for j in range(2)]
        r_aT = [Res("aT0"), Res("aT1")]
        wub = [k.sb(sF, f"wub{j}", [128, 16, 256], BF16) for j in range(2)]
        r_wub = [Res("wub0"), Res("wub1")]
        wdb = [k.sb(sF, f"wdb{j}", [128, 4, 512], BF16) for j in range(3)]
        r_wdb = [Res(f"wdb{j}") for j in range(3)]
        for j in range(2):
            for t in range(2):
                S.add("dve", lambda e, j=j, t=t: e.memset(raw[j][t][:, 0:2], 0.0), [], [r_raw[j][t]])
        TG = [(0, 512), (512, 512), (1024, 128)]
        ucnt = 0
        dcnt = 0
        qcnt = 0
        for gi in range(11):
            aj = gi % 2
            for cc in range(4):
                fc = gi * 4 + cc
                wj = fc % 2
                k.dma("pool", f"wu{wj}", wub[wj][:], dd.w_upT[fc], w=[r_wub[wj]], max_dma_last_dim=8192)
                for t in range(2):
                    for (t0, nt) in TG:
                        pb = 2 + (ucnt % 4)
                        ucnt += 1
                        tl = list(range(t0 // 128, (t0 + nt) // 128))
                        for kc in range(16):
                            k.mm(ps[pb][:, 0:nt], wub[wj][:, kc, t * 128:(t + 1) * 128], xn2T[:, kc, t0:t0 + nt], start=(kc == 0), stop=(kc == 15),
                                 r=[r_wub[wj]] + [r_xn2T[q] for q in tl], w=[psr[pb]])
                        k.cp("act", raw[wj][t][:, 2 + t0:2 + t0 + nt], ps[pb][:, 0:nt], r=[psr[pb]], w=[r_raw[wj][t]])
                    ch = fc + t * NFC
                    k.ts(yv[t][:], raw[wj][t][:, 2:2 + NOWN], cvw[:, ch, 2:3], cvw[:, ch, 3:4], ALU.mult, ALU.add,
                         r=[r_raw[wj][t], r_c3], w=[r_yv[t]])
                    k.stt(yv[t][:], raw[wj][t][:, 1:1 + NOWN], cvw[:, ch, 1:2], yv[t][:], ALU.mult, ALU.add, r=[r_raw[wj][t], r_c3, r_yv[t]], w=[r_yv[t]])
                    k.stt(yv[t][:], raw[wj][t][:, 0:NOWN], cvw[:, ch, 0:1], yv[t][:], ALU.mult, ALU.add, r=[r_raw[wj][t], r_c3, r_yv[t]], w=[r_yv[t]])
                k.af(yv[1][:, 128:NOWN], yv[1][:, 128:NOWN], AF.Silu, r=[r_yv[1]], w=[r_yv[1]])
                k.tt(aT[aj][:, cc, :], yv[1][:, 128:NOWN], yv[0][:, 128:NOWN], ALU.mult, r=[r_yv[0], r_yv[1]], w=[r_aT[aj]])
            for c4 in range(4):
                dj = dcnt % 3
                dcnt += 1
                k.dma("pool", f"wd{dj}", wdb[dj][:], dd.w_dnT[gi, c4], w=[r_wdb[dj]], max_dma_last_dim=8192)
                for i in range(1, NT):
                    pb = 6 + (qcnt % 2)
                    qcnt += 1
                    for cc in range(4):
                        k.mm(ps[pb][:, :], aT[aj][:, cc, (i - 1) * 128:i * 128], wdb[dj][:, cc, :], start=(cc == 0), stop=(cc == 3),
                             r=[r_aT[aj], r_wdb[dj]], w=[psr[pb]])
                    hv = h1[:, i, c4 * 512:(c4 + 1) * 512]
                    k.tt(hv, ps[pb][:, :], hv, ALU.add, r=[psr[pb], r_h1[i]], w=[r_h1[i]])
        S.flush(nc)
    if dbg and dbg[0] == "hh":
        k.dma("sp", "dbg", dbgd[1].rearrange("(i p) c -> p i c", p=128), h1[:], r=r_h1)
        S.flush(nc)

    with ExitStack() as sP:
        alloc_wbuf(sP)
        xn3T = k.sb(sP, "xn3T", [128, 16, 1024], BF16)
        r_xn3T = [Res(f"xn3T{i}") for i in range(8)]
        ppT = k.sb(sP, "ppT", [128, 2, 1024], BF16)
        r_ppT = [Res(f"ppT{i}") for i in range(8)]
        pf = k.sb(sP, "pf", [128, 8, 256], F32)
        pb16 = k.sb(sP, "pb16", [128, 8, 256], BF16)
        r_pf, r_pb16 = Res("pf"), Res("pb16")
        xs3 = [k.sb(sP, f"xs3{j}", [128, D], BF16) for j in range(2)]
        sq3 = [k.sb(sP, f"sq3{j}", [128, 4], F32) for j in range(2)]
        r_xs3 = [Res("xs30"), Res("xs31")]
        r_sq3 = [Res("sq30"), Res("sq31")]
        wpp = [k.sb(sP, f"wpp{j}", [128, 2, 512], BF16) for j in range(2)]
        r_wpp = [Res("wpp0"), Res("wpp1")]
        gsg = [k.sb(sP, f"gsg{j}", [128, 512], F32) for j in range(2)]
        r_gsg = [Res("gsg0"), Res("gsg1")]
        _outt = k.sb(sP, "outt", [128, D], F32)
        _routt = Res("outt")
        outt = [_outt, _outt]
        r_outt = [_routt, _routt]
        k.dma("sp", "pf", pf[:], dd.pin.rearrange("(i p) c -> p i c", p=128), w=[r_pf])
        k.cp("dve", pb16[:], pf[:], r=[r_pf], w=[r_pb16])
        alloc_gbc(sP)
        load_gain(2)
        for i in range(1, NT):
            j = i % 2
            norm_transpose((xs3[j], r_xs3[j], sq3[j], r_sq3[j]), h1[:, i, :], r_h1[i],
                           lambda q4, i=i: xn3T[:, q4 * 4:(q4 + 1) * 4, (i - 1) * 128:i * 128], [r_xn3T[i - 1]], pbase=0)
            pT = psT(6 + j, 2)
            for c2 in range(2):
                k.tr(pT[:, c2, :], pb16[:, i - 1, c2 * 128:(c2 + 1) * 128], ident[:], r=[r_pb16, r_const], w=[psr[6 + j]])
            k.cp("act", ppT[:, :, (i - 1) * 128:i * 128], pT, r=[psr[6 + j]], w=[r_ppT[i - 1]])
        gcnt = 0
        for c4 in range(4):
            wb, r_wb = load_w(dd.w_pgT[c4], (16, 512))
            wj = c4 % 2
            k.dma("pool", f"wp{wj}", wpp[wj][:], dd.w_ppT[c4], w=[r_wpp[wj]], max_dma_last_dim=8192)
            for i in range(1, NT):
                j = gcnt % 2
                gcnt += 1
                pa, pq = 2 + j, 4 + j
                tk = slice((i - 1) * 128, i * 128)
                for kc in range(16):
                    k.mm(ps[pa][:, :], xn3T[:, kc, tk], wb[:, kc, :], start=(kc == 0), stop=(kc == 15), r=[r_xn3T[i - 1], r_wb], w=[psr[pa]])
                for kc in range(2):
                    k.mm(ps[pq][:, :], ppT[:, kc, tk], wpp[wj][:, kc, :], start=(kc == 0), stop=(kc == 1), r=[r_ppT[i - 1], r_wpp[wj]], w=[psr[pq]])
                k.af(gsg[j][:], ps[pa][:, :], AF.Sigmoid, r=[psr[pa]], w=[r_gsg[j]])
                k.tt(gsg[j][:], gsg[j][:], ps[pq][:, :], ALU.mult, r=[r_gsg[j], psr[pq]], w=[r_gsg[j]])
                hv = h1[:, i, c4 * 512:(c4 + 1) * 512]
                k.tt(hv, hv, gsg[j][:], ALU.add, r=[r_gsg[j], r_h1[i]], w=[r_h1[i]])
        load_gain(3)
        for i in range(1, NT):
            j = i % 2
            rms_rstd(h1[:, i, :], D, xs3[j][:], sq3[j], r_h1[i], r_xs3[j], r_sq3[j])
            k.stt(outt[j][:], h1[:, i, :], sq3[j][:, 1:2], gbh[0][:], ALU.mult, ALU.mult, r=[r_h1[i], r_sq3[j], r_gbc], w=[r_outt[j]])
            k.dma("sp", "out0", outd[(i - 1) * 128:i * 128, :], outt[j][:], r=[r_outt[j]])
        S.flush(nc)
    sH.close()
    gs.close()
    return finish()


def _tile_w(w, ncols):
    K_, N = w.shape
    return np.ascontiguousarray(w.reshape(K_ // 128, 128, N // ncols, ncols).transpose(2, 1, 0, 3))


def host_inputs(inputs):
    f32 = np.float32
    x = np.asarray(inputs["x"], f32)
    p = np.asarray(inputs["p"], f32)[0]
    w_in = np.asarray(inputs["w_in"], f32)[0]
    sp = np.cumsum([0, 1024, 256, 256, 256, 256, 256, 256, 24, 1024, 1024, 1024])
    nq, nkc, nvc, nks, nvs, nkw, nvw, ngate, dq, dk, dv = [w_in[:, sp[i]:sp[i + 1]] for i in range(11)]
    chunks = [dq[:, 0:512], dk[:, 0:512], dv[:, 0:512], dq[:, 512:], dk[:, 512:], dv[:, 512:],
              nq[:, 0:512], nq[:, 512:], np.concatenate([nkc, nks], 1), np.concatenate([nkw, nvc], 1),
              np.concatenate([nvs, nvw], 1)]
    w_inT = np.stack([_tile_w(c, 512)[0] for c in chunks])
    w_gT = _tile_w(np.concatenate([ngate, np.zeros((D, 8), f32)], 1), 32)[0]
    gains = np.zeros((8, D), f32)
    gains[0] = inputs["attn_norm"][0]
    gains[1] = inputs["ffn_norm"][0]
    gains[2] = inputs["ple_norm"][0]
    gains[3] = inputs["final_norm"]
    gains[4, :1024] = inputs["nsa_out_norm"][0]
    gains[5, :256] = inputs["diff_subln"][0]
    lamrow = np.concatenate([inputs["diff_lq1"][0], inputs["diff_lk1"][0], inputs["diff_lq2"][0], inputs["diff_lk2"][0]])[None, :].astype(f32)
    cw = np.asarray(inputs["conv_w"], f32)[0]
    cb = np.asarray(inputs["conv_b"], f32)[0]
    convw = np.ascontiguousarray(np.stack([cw[0], cw[1], cw[2], cb], axis=-1).reshape(2 * NFC, 128, 4).transpose(1, 0, 2))
    cw1T = np.stack([np.asarray(inputs[n][0], f32).reshape(32, 128, 256).transpose(1, 0, 2) for n in ("cmp_k_w1", "cmp_v_w1")])
    cw2T = np.stack([np.asarray(inputs[n][0], f32).reshape(2, 128, 128).transpose(1, 0, 2) for n in ("cmp_k_w2", "cmp_v_w2")])
    cposT = np.stack([np.asarray(inputs["cmp_k_pos"][0], f32).T, np.asarray(inputs["cmp_v_pos"][0], f32).T], axis=1)
    w_up = np.asarray(inputs["w_up"], f32)[0]
    w_upT = np.ascontiguousarray(
        np.concatenate([w_up[:, :D_FF].reshape(16, 128, NFC, 1, 128), w_up[:, D_FF:].reshape(16, 128, NFC, 1, 128)], axis=3)
        .transpose(2, 1, 0, 3, 4).reshape(NFC, 128, 16, 256))
    w_dn = np.asarray(inputs["w_down"], f32)[0]
    w_dnT = np.ascontiguousarray(w_dn.reshape(11, 4, 128, 4, 512).transpose(0, 3, 2, 1, 4))
    kk = np.arange(128)
    tri = (kk[:, None] <= kk[None, :]).astype(f32)
    tri2 = np.concatenate([tri, 1.0 - tri], axis=1).astype(ml_dtypes.bfloat16)
    ident = np.eye(128, dtype=f32).astype(ml_dtypes.bfloat16)
    cstart = np.arange(127) * 16
    sstart = np.arange(32) * 64
    overlap = (np.clip(np.minimum(cstart[:, None] + 32, sstart[None, :] + 64) - np.maximum(cstart[:, None], sstart[None, :]), 0, None) / 32.0).astype(f32)
    own_tok = 7 * 128 + np.arange(NOWN)
    cmpmask = (cstart[:, None] + 31 <= own_tok[None, :]).astype(f32).reshape(127, NT, 128).astype(ml_dtypes.bfloat16)
    expand = np.zeros((128, NSB, 128), f32)
    for s in range(NSB):
        for kq in range(128):
            expand[2 * s + kq // 64, s, kq] = 1.0
    expand = expand.astype(ml_dtypes.bfloat16)
    inv = (1.0 / (500000.0 ** (np.arange(0, 32, 2, dtype=f32) / f32(32.0)))).astype(f32)
    shared = dict(w_inT=w_inT, w_gT=np.ascontiguousarray(w_gT), gains=gains, lamrow=lamrow, convw=convw,
                  cw1T=np.ascontiguousarray(cw1T), cw2T=np.ascontiguousarray(cw2T), cposT=np.ascontiguousarray(cposT),
                  w_oT=_tile_w(np.asarray(inputs["w_o"], f32)[0], 512), w_upT=w_upT, w_dnT=w_dnT,
                  w_pgT=_tile_w(np.asarray(inputs["w_ple_gate"], f32)[0], 512),
                  w_ppT=_tile_w(np.asarray(inputs["w_ple_proj"], f32)[0], 512),
                  tri=tri2, ident=ident, overlap=overlap, cmpmask=cmpmask, expand=expand)
    maps = []
    for c in range(8):
        b, hh = c // 2, c % 2
        off = 0 if hh == 1 else -1024
        pos = np.arange(NSB * 128) + off
        real = pos >= 0
        if hh == 1:
            xe = x[b]
            pin = p[b, 1024:2048]
        else:
            xe = np.concatenate([np.zeros((1024, D), f32), x[b, :1024]], axis=0)
            pin = p[b, 0:1024]
        ang = (np.maximum(pos, 0).astype(f32))[:, None] * inv[None, :]
        cos = np.where(real[:, None], np.cos(ang), 1.0).astype(f32)
        sin = np.where(real[:, None], np.sin(ang), 0.0).astype(f32)
        cst = np.concatenate([cos, cos, sin, sin], axis=1)
        valid = np.ascontiguousarray(real.astype(f32).reshape(NSB, 128).T)
        validc = ((cstart + off) >= 0).astype(f32)[:, None]
        t_real = own_tok + off
        cur = np.floor_divide(t_real, 64)
        jr = np.arange(32) + off // 64
        forced = (jr[None, :] == 0) | (jr[None, :] == cur[:, None]) | (jr[None, :] == cur[:, None] - 1)
        future = (jr[None, :] > cur[:, None]) | (jr[None, :] < 0)
        sadd = np.where(forced, 1e4, np.where(future, -1e4, 0.0)).astype(f32)
        smul = np.where(forced | future, 0.0, 1.0).astype(f32)
        m = dict(shared)
        m.update(xe=np.ascontiguousarray(xe), pin=np.ascontiguousarray(pin), cst=cst, valid=valid, validc=validc,
                 smul=np.ascontiguousarray(smul.reshape(NT, 128, 32).transpose(1, 0, 2)),
                 sadd=np.ascontiguousarray(sadd.reshape(NT, 128, 32).transpose(1, 0, 2)))
        maps.append(m)
    return maps


def kernel(**inputs):
    maps = host_inputs(inputs)
    nc = build_program()
    res = run_bass_kernel_spmd(nc, maps, core_ids=list(range(8)))
    out = np.zeros((4, 2048, D), np.float32)
    for c in range(8):
        b, hh = c // 2, c % 2
        out[b, hh * 1024:(hh + 1) * 1024] = res.results[c]["out"]
    return out
```
